# Optimizing a Trainium2 kernel written in Bass

```python
import numpy as np
import jax
import jax.numpy as jnp
from jax import lax

D_MODEL = 2048
BATCH = 4
SEQ = 2048
DEPTH = 4

GRID_W = 64
CTX_LEN = 256
HEAD_DIM = 128
N_HEADS_A = D_MODEL // (2 * HEAD_DIM)
N_HEADS_B = D_MODEL // (2 * HEAD_DIM)
WIDTH_A = N_HEADS_A * HEAD_DIM
WIDTH_B = N_HEADS_B * HEAD_DIM
N_GATES = 4 * N_HEADS_B
SPLIT_EVEN = (WIDTH_A, 2 * WIDTH_A, 3 * WIDTH_A,
              3 * WIDTH_A + WIDTH_B, 3 * WIDTH_A + 2 * WIDTH_B,
              3 * WIDTH_A + 3 * WIDTH_B, 3 * WIDTH_A + 4 * WIDTH_B)
IN_EVEN = 3 * WIDTH_A + 4 * WIDTH_B + N_GATES
WIN_ROWS = 8
WIN_COLS = 16
Q_COL_BLOCK = 16
K_COL_BLOCK = 32
ATT_SCALE = HEAD_DIM ** -0.5
MLSTM_CHUNK = 64
MLSTM_CONV = 3
SGU_CHUNK = 128
SGU_GROUPS = 8
SGU_WIDTH = 2 * D_MODEL
N_GROUPS = 4
EXPERTS_PER_GROUP = 8
N_EXPERTS = N_GROUPS * EXPERTS_PER_GROUP
TOP_K_IN_GROUP = 2
D_EXPERT = D_MODEL // 4
N_EVEN = (DEPTH + 1) // 2
N_ODD = DEPTH // 2
EPS = 1e-6
NEG = -1e30
F32 = jnp.float32

kernel_name = "hybrid_natten_mlstm_sgu_hmoe_diffusion"


def rms_norm(x, gain):
    xf = x.astype(F32)
    y = xf * lax.rsqrt(jnp.mean(xf * xf, axis=-1, keepdims=True) + EPS)
    return (y * gain.astype(F32)).astype(x.dtype)


def modulate(x, gain, shift, scale):
    return rms_norm(x, gain) * (1 + scale) + shift


def split_heads(t, n_heads):
    b, t_len, _ = t.shape
    return t.reshape(b, t_len, n_heads, -1).transpose(0, 2, 1, 3)


def merge_heads(t):
    b, h, t_len, d = t.shape
    return t.transpose(0, 2, 1, 3).reshape(b, t_len, h * d)


def centred_dwconv(x, w):
    k = w.shape[0]
    pad = k // 2
    return lax.conv_general_dilated(
        x, w.astype(x.dtype)[:, None, :], window_strides=(1,), padding=[(pad, k - 1 - pad)],
        dimension_numbers=('NWC', 'WIO', 'NWC'), feature_group_count=x.shape[-1])


def neighbourhood_attention(q, k, v, k_ctx, v_ctx, rpb):
    b, h, n, dh = q.shape
    rows = n // GRID_W
    kr = min(WIN_ROWS, rows)
    nb = GRID_W // Q_COL_BLOCK
    q_col = np.arange(GRID_W).reshape(nb, Q_COL_BLOCK)
    k_col = (np.clip(np.arange(nb) * Q_COL_BLOCK - WIN_COLS // 2, 0, GRID_W - K_COL_BLOCK)[:, None]
             + np.arange(K_COL_BLOCK))
    w_start = np.clip(q_col - WIN_COLS // 2, 0, GRID_W - WIN_COLS)
    rel = k_col[:, None, :] - w_start[:, :, None]
    in_win = (rel >= 0) & (rel < WIN_COLS)
    dc_idx = np.clip(k_col[:, None, :] - q_col[:, :, None] + WIN_COLS - 1, 0, 2 * WIN_COLS - 2)
    n_loc = kr * K_COL_BLOCK
    mask = np.broadcast_to(in_win[:, :, None, :], (nb, Q_COL_BLOCK, kr, K_COL_BLOCK)).reshape(
        nb, Q_COL_BLOCK, n_loc)
    k_grid = k.reshape(b, h, rows, GRID_W, dh)
    v_grid = v.reshape(b, h, rows, GRID_W, dh)
    q_rows = jnp.moveaxis(q.reshape(b, h, rows, nb, Q_COL_BLOCK, dh), 2, 0)

    def attend_row(args):
        r, q_r = args
        r0 = jnp.clip(r - kr // 2, 0, rows - kr)
        k_blk = lax.dynamic_slice_in_dim(k_grid, r0, kr, axis=2)[:, :, :, k_col]
        v_blk = lax.dynamic_slice_in_dim(v_grid, r0, kr, axis=2)[:, :, :, k_col]
        k_blk = jnp.moveaxis(k_blk, 3, 2).reshape(b, h, nb, n_loc, dh)
        v_blk = jnp.moveaxis(v_blk, 3, 2).reshape(b, h, nb, n_loc, dh)
        dr_idx = r0 + jnp.arange(kr) - r + WIN_ROWS - 1
        bias = rpb[:, dr_idx][:, :, dc_idx]
        bias = jnp.transpose(bias, (0, 2, 3, 1, 4)).reshape(h, nb, Q_COL_BLOCK, n_loc)
        s_loc = jnp.einsum('bhnqd,bhnkd->bhnqk', q_r, k_blk).astype(F32) + bias.astype(F32)
        s_loc = jnp.where(mask, s_loc, NEG)
        s_ctx = jnp.einsum('bhnqd,bhkd->bhnqk', q_r, k_ctx).astype(F32)
        p = jax.nn.softmax(jnp.concatenate([s_loc, s_ctx], axis=-1), axis=-1).astype(v.dtype)
        return (jnp.einsum('bhnqk,bhnkd->bhnqd', p[..., :n_loc], v_blk)
                + jnp.einsum('bhnqk,bhkd->bhnqd', p[..., n_loc:], v_ctx))

    out = lax.map(attend_row, (jnp.arange(rows), q_rows))
    return jnp.moveaxis(out, 0, 2).reshape(b, h, n, dh)


def context_attention(q, k, v):
    p = jax.nn.softmax(jnp.einsum('bhqd,bhkd->bhqk', q, k).astype(F32), axis=-1).astype(v.dtype)
    return jnp.einsum('bhqk,bhkd->bhqd', p, v)


def mlstm_zero_state(batch):
    return (jnp.zeros((batch, N_HEADS_B, HEAD_DIM, HEAD_DIM), F32),
            jnp.zeros((batch, N_HEADS_B, HEAD_DIM), F32),
            jnp.zeros((batch, N_HEADS_B), F32))


def mlstm_chunk_states(k, v, ig, lf, init):
    b, h, t_len, d = k.shape
    nc = t_len // MLSTM_CHUNK
    kc = k.reshape(b, h, nc, MLSTM_CHUNK, d).astype(F32)
    vc = v.reshape(b, h, nc, MLSTM_CHUNK, d).astype(F32)
    cum = jnp.cumsum(lf.reshape(b, h, nc, MLSTM_CHUNK), axis=-1)
    a = cum[..., -1:] - cum + ig.reshape(b, h, nc, MLSTM_CHUNK)
    m_loc = jnp.max(a, axis=-1)
    w = jnp.exp(a - m_loc[..., None])
    c_loc = jnp.einsum('bhcs,bhcsk,bhcsv->bhckv', w, kc, vc)
    n_loc = jnp.einsum('bhcs,bhcsk->bhck', w, kc)
    b_tot = cum[..., -1]

    def step(carry, xs):
        c_st, n_st, m_st = carry
        cl, nl, ml, bt = xs
        m_new = jnp.maximum(bt + m_st, ml)
        f_old = jnp.exp(bt + m_st - m_new)
        f_loc = jnp.exp(ml - m_new)
        c_new = f_old[..., None, None] * c_st + f_loc[..., None, None] * cl
        n_new = f_old[..., None] * n_st + f_loc[..., None] * nl
        return (c_new, n_new, m_new), (c_st, n_st, m_st)

    xs = tuple(jnp.moveaxis(t, 2, 0) for t in (c_loc, n_loc, m_loc, b_tot))
    final, prev = lax.scan(step, init, xs)
    prev = tuple(jnp.moveaxis(t, 0, 2) for t in prev)
    return prev, final


def mlstm_chunk_outputs(q, k, v, ig, lf, prev):
    c_prev, n_prev, m_prev = prev
    b, h, t_len, d = q.shape
    nc = t_len // MLSTM_CHUNK
    qc = q.reshape(b, h, nc, MLSTM_CHUNK, d).astype(F32)
    kc = k.reshape(b, h, nc, MLSTM_CHUNK, d).astype(F32)
    vc = v.reshape(b, h, nc, MLSTM_CHUNK, d).astype(F32)
    cum = jnp.cumsum(lf.reshape(b, h, nc, MLSTM_CHUNK), axis=-1)
    igc = ig.reshape(b, h, nc, MLSTM_CHUNK)
    lower = np.tril(np.ones((MLSTM_CHUNK, MLSTM_CHUNK), dtype=bool))
    dmat = jnp.where(lower, cum[..., :, None] - cum[..., None, :] + igc[..., None, :], NEG)
    inter = cum + m_prev[..., None]
    m_t = jnp.maximum(inter, jnp.max(dmat, axis=-1))
    wts = jnp.exp(dmat - m_t[..., None]) * jnp.einsum('bhctd,bhcsd->bhcts', qc, kc)
    g = jnp.exp(inter - m_t)
    num = (jnp.einsum('bhcts,bhcsv->bhctv', wts, vc)
           + g[..., None] * jnp.einsum('bhctk,bhckv->bhctv', qc, c_prev))
    den = jnp.sum(wts, axis=-1) + g * jnp.einsum('bhctk,bhck->bhct', qc, n_prev)
    out = num / jnp.maximum(jnp.abs(den), jnp.exp(-m_t))[..., None]
    return out.reshape(b, h, t_len, d)


def _flip(t):
    return jnp.flip(t, axis=2)


def mlstm_bidir_states(k, v, gates, init_f, init_b):
    ig_f, lf_f, ig_b, lf_b = gates
    prev_f, fin_f = mlstm_chunk_states(k, v, ig_f, lf_f, init_f)
    prev_b, fin_b = mlstm_chunk_states(_flip(k), _flip(v), _flip(ig_b), _flip(lf_b), init_b)
    return prev_f, prev_b, fin_f, fin_b


def mlstm_bidir_outputs(q, k, v, gates, prev_f, prev_b):
    ig_f, lf_f, ig_b, lf_b = gates
    h_f = mlstm_chunk_outputs(q, k, v, ig_f, lf_f, prev_f)
    h_b = mlstm_chunk_outputs(_flip(q), _flip(k), _flip(v), _flip(ig_b), _flip(lf_b), prev_b)
    return h_f + _flip(h_b)


def mlstm_readout(h, o, head_gain):
    h = rms_norm(h, head_gain.reshape(1, N_HEADS_B, 1, HEAD_DIM))
    return merge_heads(h).astype(o.dtype) * jax.nn.sigmoid(o)


def even_project(t, w_in, conv_w, gate_b, qk_gain):
    p = jnp.dot(t, w_in)
    qa, ka, va, qb, kb, vb, ob, g = jnp.split(p, SPLIT_EVEN, axis=-1)
    qa = rms_norm(split_heads(qa, N_HEADS_A), qk_gain[0]) * ATT_SCALE
    ka = rms_norm(split_heads(ka, N_HEADS_A), qk_gain[1])
    va = split_heads(va, N_HEADS_A)
    qk = jax.nn.silu(centred_dwconv(jnp.concatenate([qb, kb], axis=-1), conv_w))
    qb, kb = jnp.split(qk, 2, axis=-1)
    qb = split_heads(qb, N_HEADS_B)
    kb = split_heads(kb, N_HEADS_B) * (HEAD_DIM ** -0.5)
    vb = split_heads(vb, N_HEADS_B)
    g = (g.astype(F32) + gate_b.astype(F32)).transpose(0, 2, 1)
    ig_f, fg_f, ig_b, fg_b = jnp.split(g, 4, axis=1)
    gates = (ig_f, jax.nn.log_sigmoid(fg_f), ig_b, jax.nn.log_sigmoid(fg_b))
    return qa, ka, va, qb, kb, vb, ob, gates


def even_mixer(xm, cm, w_in, conv_w, gate_b, qk_gain, rpb, head_gain, w_out, need_ctx):
    batch = xm.shape[0]
    qa, ka, va, qb, kb, vb, ob, gates = even_project(xm, w_in, conv_w, gate_b, qk_gain)
    qac, kac, vac, qbc, kbc, vbc, obc, gates_c = even_project(cm, w_in, conv_w, gate_b, qk_gain)
    ya = neighbourhood_attention(qa, ka, va, kac, vac, rpb)
    zero = mlstm_zero_state(batch)
    prev_cf, prev_cb, fin_cf, fin_cb = mlstm_bidir_states(kbc, vbc, gates_c, zero, zero)
    prev_f, prev_b, _, _ = mlstm_bidir_states(kb, vb, gates, fin_cf, fin_cb)
    yb = mlstm_readout(mlstm_bidir_outputs(qb, kb, vb, gates, prev_f, prev_b), ob, head_gain)
    y = jnp.dot(jnp.concatenate([merge_heads(ya), yb], axis=-1), w_out)
    if not need_ctx:
        return y, None
    yac = context_attention(qac, kac, vac)
    ybc = mlstm_readout(mlstm_bidir_outputs(qbc, kbc, vbc, gates_c, prev_cf, prev_cb), obc, head_gain)
    yc = jnp.dot(jnp.concatenate([merge_heads(yac), ybc], axis=-1), w_out)
    return y, yc


def sgu_mixer(x, w_in, v_gain, w_s, b_s, w_out):
    b, t_len, _ = x.shape
    nc = t_len // SGU_CHUNK
    u, v = jnp.split(jax.nn.gelu(jnp.dot(x, w_in)), 2, axis=-1)
    v = rms_norm(v, v_gain).reshape(b, nc, SGU_CHUNK, SGU_GROUPS, SGU_WIDTH // SGU_GROUPS)
    s = jnp.einsum('gts,bcsgd->bctgd', w_s, v) + b_s.T[None, None, :, :, None]
    return jnp.dot(u * s.reshape(b, t_len, SGU_WIDTH), w_out)


def hierarchical_moe(t, w_rg, b_rg, w_re, b_re, w_gate, w_up, w_down):
    n_tok = t.shape[0]
    lg = jnp.dot(t, w_rg).astype(F32) + b_rg.astype(F32)
    top_g, g_sel = lax.top_k(lg, 1)
    p_group = jnp.exp(top_g[:, 0] - jax.nn.logsumexp(lg, axis=-1))
    le = (jnp.dot(t, w_re).astype(F32) + b_re.astype(F32)).reshape(n_tok, N_GROUPS, EXPERTS_PER_GROUP)
    le_sel = jnp.take_along_axis(le, g_sel[:, :, None], axis=1)[:, 0]
    top_e, e_sel = lax.top_k(le_sel, TOP_K_IN_GROUP)
    p_exp = jax.nn.softmax(top_e, axis=-1)
    expert_id = g_sel * EXPERTS_PER_GROUP + e_sel
    combine = jnp.einsum('tk,tke->te', p_group[:, None] * p_exp,
                         jax.nn.one_hot(expert_id, N_EXPERTS, dtype=F32))
    out = jnp.zeros(t.shape, F32)
    for g in range(N_GROUPS):
        sl = slice(g * EXPERTS_PER_GROUP, (g + 1) * EXPERTS_PER_GROUP)
        hid = (jax.nn.silu(jnp.einsum('td,edf->etf', t, w_gate[sl]))
               * jnp.einsum('td,edf->etf', t, w_up[sl])
               * combine[:, sl].T[:, :, None].astype(t.dtype))
        out = out + jnp.einsum('etf,efd->td', hid, w_down[sl]).astype(F32)
    return out.astype(t.dtype)


def setup_inputs(seed: int = 0) -> dict:
    key = jax.random.key(seed)
    keys = iter(jax.random.split(key, 40))

    def nrm(shape, scale):
        return jax.random.normal(next(keys), shape, F32) * scale

    d = D_MODEL
    forget_bias = jnp.linspace(3.0, 6.0, N_HEADS_B, dtype=F32)
    inputs = {}
    inputs["x"] = nrm((BATCH, SEQ, d), 1.0)
    inputs["c"] = nrm((BATCH, d), 1.0)
    inputs["ctx"] = nrm((BATCH, CTX_LEN, d), 1.0)
    inputs["c_ctx"] = nrm((d,), 1.0)
    inputs["w_mod"] = nrm((DEPTH, d, 6 * d), 0.5 * d ** -0.5)
    inputs["b_mod"] = nrm((DEPTH, 6 * d), 0.02)
    inputs["norm_gain"] = 1.0 + nrm((DEPTH, 2, d), 0.02)
    inputs["even_w_in"] = nrm((N_EVEN, d, IN_EVEN), d ** -0.5)
    inputs["even_conv"] = nrm((N_EVEN, MLSTM_CONV, 2 * WIDTH_B), MLSTM_CONV ** -0.5)
    inputs["even_gate_b"] = jnp.concatenate([
        nrm((N_EVEN, N_HEADS_B), 0.1), forget_bias + nrm((N_EVEN, N_HEADS_B), 0.1),
        nrm((N_EVEN, N_HEADS_B), 0.1), forget_bias + nrm((N_EVEN, N_HEADS_B), 0.1)], axis=-1)
    inputs["even_qk_gain"] = 1.0 + nrm((N_EVEN, 2, HEAD_DIM), 0.02)
    inputs["even_rpb"] = nrm((N_EVEN, N_HEADS_A, 2 * WIN_ROWS - 1, 2 * WIN_COLS - 1), 0.1)
    inputs["even_head_gain"] = 1.0 + nrm((N_EVEN, WIDTH_B), 0.02)
    inputs["even_w_out"] = nrm((N_EVEN, WIDTH_A + WIDTH_B, d), (WIDTH_A + WIDTH_B) ** -0.5)
    inputs["odd_w_in"] = nrm((N_ODD, d, 2 * SGU_WIDTH), d ** -0.5)
    inputs["odd_v_gain"] = 1.0 + nrm((N_ODD, SGU_WIDTH), 0.02)
    inputs["odd_w_s"] = nrm((N_ODD, SGU_GROUPS, SGU_CHUNK, SGU_CHUNK), SGU_CHUNK ** -0.5)
    inputs["odd_b_s"] = 1.0 + nrm((N_ODD, SGU_GROUPS, SGU_CHUNK), 0.02)
    inputs["odd_w_out"] = nrm((N_ODD, SGU_WIDTH, d), SGU_WIDTH ** -0.5)
    inputs["moe_w_rg"] = nrm((DEPTH, d, N_GROUPS), d ** -0.5)
    inputs["moe_b_rg"] = nrm((DEPTH, N_GROUPS), 0.01)
    inputs["moe_w_re"] = nrm((DEPTH, d, N_EXPERTS), d ** -0.5)
    inputs["moe_b_re"] = nrm((DEPTH, N_EXPERTS), 0.01)
    inputs["moe_w_gate"] = nrm((DEPTH, N_EXPERTS, d, D_EXPERT), d ** -0.5)
    inputs["moe_w_up"] = nrm((DEPTH, N_EXPERTS, d, D_EXPERT), d ** -0.5)
    inputs["moe_w_down"] = nrm((DEPTH, N_EXPERTS, D_EXPERT, d), D_EXPERT ** -0.5)
    return inputs


def reference(x, c, ctx, c_ctx, w_mod, b_mod, norm_gain, even_w_in, even_conv, even_gate_b,
              even_qk_gain, even_rpb, even_head_gain, even_w_out, odd_w_in, odd_v_gain, odd_w_s,
              odd_b_s, odd_w_out, moe_w_rg, moe_b_rg, moe_w_re, moe_b_re, moe_w_gate, moe_w_up,
              moe_w_down):
    b, n, d = x.shape
    n_ctx = ctx.shape[1]
    s_lat = jax.nn.silu(c)
    s_ctx = jax.nn.silu(c_ctx)
    h, hc = x, ctx
    for layer in range(DEPTH):
        i = layer // 2
        is_even = layer % 2 == 0
        need_ctx = any(j % 2 == 0 for j in range(layer + 1, DEPTH))
        use_ctx = need_ctx or is_even
        sh_a, sc_a, g_a, sh_f, sc_f, g_f = jnp.split(
            (jnp.dot(s_lat, w_mod[layer]) + b_mod[layer])[:, None, :], 6, axis=-1)
        xm = modulate(h, norm_gain[layer, 0], sh_a, sc_a)
        if use_ctx:
            csh_a, csc_a, cg_a, csh_f, csc_f, cg_f = jnp.split(
                jnp.dot(s_ctx, w_mod[layer]) + b_mod[layer], 6, axis=-1)
            cm = modulate(hc, norm_gain[layer, 0], csh_a, csc_a)
        if is_even:
            y, yc = even_mixer(xm, cm, even_w_in[i], even_conv[i], even_gate_b[i], even_qk_gain[i],
                               even_rpb[i], even_head_gain[i], even_w_out[i], need_ctx)
        else:
            y = sgu_mixer(xm, odd_w_in[i], odd_v_gain[i], odd_w_s[i], odd_b_s[i], odd_w_out[i])
            if need_ctx:
                yc = sgu_mixer(cm, odd_w_in[i], odd_v_gain[i], odd_w_s[i], odd_b_s[i], odd_w_out[i])
        h = h + g_a * y.astype(h.dtype)
        xm2 = modulate(h, norm_gain[layer, 1], sh_f, sc_f)
        moe_args = (moe_w_rg[layer], moe_b_rg[layer], moe_w_re[layer], moe_b_re[layer],
                    moe_w_gate[layer], moe_w_up[layer], moe_w_down[layer])
        if need_ctx:
            hc = hc + cg_a * yc.astype(hc.dtype)
            cm2 = modulate(hc, norm_gain[layer, 1], csh_f, csc_f)
            tok = jnp.concatenate([xm2.reshape(b * n, d), cm2.reshape(b * n_ctx, d)], axis=0)
            out = hierarchical_moe(tok, *moe_args)
            hc = hc + cg_f * out[b * n:].reshape(b, n_ctx, d)
            y2 = out[:b * n].reshape(b, n, d)
        else:
            y2 = hierarchical_moe(xm2.reshape(b * n, d), *moe_args).reshape(b, n, d)
        h = h + g_f * y2
    return h
```

```python
import numpy as np
import concourse.bass as bass
import concourse.mybir as mybir
from concourse.bass_utils import run_bass_kernel_spmd

F32 = mybir.dt.float32
BF16 = mybir.dt.bfloat16
AF = mybir.ActivationFunctionType
ALU = mybir.AluOpType
AX = mybir.AxisListType

D = 2048
NCH = 16
NL = 2048
NCX = 256
NT = NL + NCX
DEPTH = 4
EPS = 1e-6
NEG = -1e30
NDS = 40
NLOC = 1152
I32 = mybir.dt.int32


class Tok:
    __slots__ = ("sem", "key", "val", "eng")

    def __init__(self, sem, key, val, eng):
        self.sem, self.key, self.val, self.eng = sem, key, val, eng


class Buf:
    __slots__ = ("name", "w", "r")

    def __init__(self, name):
        self.name, self.w, self.r = name, None, {}


class KB:
    def __init__(self):
        nc = bass.Bass("TRN2", target_bir_lowering=False, num_devices=8)
        self.nc = nc
        self.eng = {"pe": nc.tensor, "act": nc.scalar, "dve": nc.vector, "pool": nc.gpsimd, "sp": nc.sync}
        self.sem = {k: nc.alloc_semaphore("s_" + k) for k in ("pe", "act", "dve", "pool")}
        self.cnt = {k: 0 for k in self.eng}
        self.seen = {k: {} for k in self.eng}
        self.dsem = [nc.alloc_semaphore("d%d" % i) for i in range(NDS)]
        self.dcnt = [0] * NDS
        self.dnext = 0
        self.nbuf = 0

    def buf(self, name=None):
        self.nbuf += 1
        return Buf(name or "b%d" % self.nbuf)

    def _waits(self, e, reads, writes, is_dma):
        waits = {}

        def need(tok, raw):
            if tok is None:
                return
            if (not is_dma) and tok.eng == e:
                if e == "pe" or not raw:
                    return
                if self.cnt[e] - tok.val >= 6:
                    return
            cur = waits.get(tok.key)
            if cur is None or cur.val < tok.val:
                waits[tok.key] = tok

        for b in reads:
            need(b.w, True)
        for b in writes:
            need(b.w, False)
            for t in b.r.values():
                need(t, False)
        E = self.eng[e]
        seen = self.seen[e]
        for key, tok in waits.items():
            if seen.get(key, 0) >= tok.val:
                continue
            E.wait_ge(tok.sem, tok.val)
            seen[key] = tok.val

    def _mark(self, tok, reads, writes):
        for b in writes:
            b.w = tok
            b.r = {}
        for b in reads:
            if b in writes:
                continue
            cur = b.r.get(tok.key)
            if cur is None or cur.val < tok.val:
                b.r[tok.key] = tok

    def op(self, e, reads, writes, fn):
        self._waits(e, reads, writes, False)
        ins = fn(self.eng[e])
        self.cnt[e] += 1
        ins.then_inc(self.sem[e], 1)
        tok = Tok(self.sem[e], e, self.cnt[e], e)
        self._mark(tok, reads, writes)
        return tok

    def dma(self, q, out, in_, reads, writes, **kw):
        j = self.dnext
        self.dnext = (self.dnext + 1) % NDS
        E = self.eng[q]
        key = "d%d" % j
        if self.dcnt[j] > 0 and self.seen[q].get(key, 0) < self.dcnt[j]:
            E.wait_ge(self.dsem[j], self.dcnt[j])
            self.seen[q][key] = self.dcnt[j]
        self._waits(q, reads, writes, True)
        if callable(out) or callable(in_):
            assert q == "sp"
            for sl in (0, 1):
                self.prog.slot = sl
                o = out() if callable(out) else out
                i = in_() if callable(in_) else in_
                guard = E.If_eq(self.slot_reg, 0) if sl == 0 else E.Else()
                with guard:
                    E.dma_start(out=o, in_=i, **kw).then_inc(self.dsem[j], 16)
            self.prog.slot = None
        else:
            ins = E.dma_start(out=out, in_=in_, **kw)
            ins.then_inc(self.dsem[j], 16)
        self.dcnt[j] += 16
        tok = Tok(self.dsem[j], key, self.dcnt[j], None)
        self._mark(tok, reads, writes)
        return tok

    def barrier(self):
        for e in self.eng:
            E = self.eng[e]
            for o in ("pe", "act", "dve", "pool"):
                if o != e and self.cnt[o] > self.seen[e].get(o, 0):
                    E.wait_ge(self.sem[o], self.cnt[o])
                    self.seen[e][o] = self.cnt[o]
            for j in range(NDS):
                key = "d%d" % j
                if self.dcnt[j] > self.seen[e].get(key, 0):
                    E.wait_ge(self.dsem[j], self.dcnt[j])
                    self.seen[e][key] = self.dcnt[j]

    def pair_barrier(self, flags, nonce, slot, other):
        nc = self.nc
        sp = self.eng["sp"]
        if not hasattr(self, "bar_sem"):
            self.bar_sem = nc.alloc_semaphore("pairbar")
            self.bar_cnt = 0
            self.bar_regs = (sp.alloc_register("nreg"), sp.alloc_register("treg"), sp.alloc_register("creg"))
            sp.load(self.bar_regs[0], nonce[0:1, 0:1])
        nreg, treg, creg = self.bar_regs
        for o in ("pe", "act", "dve", "pool"):
            if self.cnt[o] > self.seen["sp"].get(o, 0):
                sp.wait_ge(self.sem[o], self.cnt[o])
                self.seen["sp"][o] = self.cnt[o]
        for j in range(NDS):
            key = "d%d" % j
            if self.dcnt[j] > self.seen["sp"].get(key, 0):
                sp.wait_ge(self.dsem[j], self.dcnt[j])
                self.seen["sp"][key] = self.dcnt[j]
        ev = self.bar_cnt
        for sl in (0, 1):
            guard = sp.If_eq(self.slot_reg, 0) if sl == 0 else sp.Else()
            with guard:
                sp.store(flags[ev, sl:sl + 1, 0:1], nreg)
                sp.reg_mov(creg, 1)
                with sp.While(creg):
                    sp.load(treg, flags[ev, 1 - sl:2 - sl, 0:1])
                    sp.reg_sub(creg, treg, nreg)
        self.bar_cnt += 1
        sp.sem_inc(self.bar_sem, 1)
        for e in ("pe", "act", "dve", "pool"):
            self.eng[e].wait_ge(self.bar_sem, self.bar_cnt)

    def final_wait(self, toks):
        E = self.eng["sp"]
        for t in toks:
            E.wait_ge(t.sem, t.val)


class Prog:
    def __init__(self, upto="all"):
        self.kb = KB()
        self.nc = self.kb.nc
        self.upto = upto
        self.T = {}

    def un(self, name):
        self._uid = getattr(self, "_uid", 0) + 1
        return "%s_%d" % (name, self._uid)

    def din(self, name, shape, dt=F32):
        t = self.nc.dram_tensor(name, list(shape), dt, kind="ExternalInput").ap()
        self.T[name] = t
        return t

    def dscratch(self, name, shape, dt):
        return self.nc.dram_tensor(name, list(shape), dt, kind="Internal").ap()

    def sb(self, name, shape, dt):
        return self.nc.alloc_sbuf_tensor(name, list(shape), dt)

    def dshared(self, name, shape, dt):
        return self.nc.dram_tensor(name, list(shape), dt, kind="Internal", addr_space="Shared").ap()

    def tk(self, ap, gi):
        nd = len(ap.shape)
        pre = "p c" if nd == 3 else "p"
        lead = (slice(None),) * (nd - 1)
        if gi == 2:
            v = ap[lead + (slice(NL, NT),)].rearrange("%s (s t) -> %s s t" % (pre, pre), s=2)
            v = v[lead + (bass.ts(self.slot, 1),)]
            return v.rearrange("%s s t -> %s (s t)" % (pre, pre))
        v = ap[lead + (slice(0, NL),)].rearrange("%s (s t) -> %s s t" % (pre, pre), s=2)
        v = v[lead + (bass.ts(self.slot, 1),)]
        if gi == "lat":
            return v.rearrange("%s s t -> %s (s t)" % (pre, pre))
        v = v[lead + (slice(None), slice(gi * 512, (gi + 1) * 512))]
        return v.rearrange("%s s t -> %s (s t)" % (pre, pre))

    def hd3(self, ap, w, hh):
        v = ap.rearrange("(w s e) p t -> w s e p t", w=2, s=2, e=4)[w, bass.ts(self.slot, 1), hh]
        return v.rearrange("s p t -> p (s t)")

    def vcols(self, a, hh):
        v = self.V_d.rearrange("(T p) (a s e j) -> p T a s e j", p=128, a=3, s=2, e=4)[:, :, a, bass.ts(self.slot, 1), hh, :]
        return v.rearrange("p T s j -> p T (s j)")

    def pbar(self):
        self.kb.barrier()
        self.kb.pair_barrier(self.flags, self.nonce, self.slot_rt, self.other_rt)

    def mm(self, out, lhsT, rhs, start, stop, reads, writes):
        return self.kb.op("pe", reads, writes, lambda E: E.matmul(out, lhsT, rhs, start=start, stop=stop))

    def build(self):
        nc, kb = self.nc, self.kb
        xT = self.din("xT", [128, NCH, NLOC])
        self.nonce = self.din("nonce", [1, 4], I32)
        self.flags = self.dshared("flags", [8, 2, 4], I32)
        pid = nc.sync.partition_id()
        self.slot_rt = pid % 2
        self.other_rt = None
        kb.slot_reg = nc.sync.to_reg(self.slot_rt)
        kb.prog = self
        self.slot = None
        cT = self.din("cT", [128, NCH, 2])
        w_mod = self.din("w_mod", [DEPTH, D, 6 * D])
        b_mod = self.din("b_mod", [DEPTH, 6 * D])
        ngT = self.din("ngT", [128, DEPTH, 2, NCH])
        out = nc.dram_tensor("out", [128, NCH, 1024], F32, kind="ExternalOutput").ap()
        self.out = out
        hT_d = self.dshared("hT_d", [128, NCH, NT], F32)
        self.hT_d = hT_d
        self.b_hT = [kb.buf("hT_d%d" % i) for i in range(3)]
        self.LG = [(0, 512, 0, 0), (512, 512, 0, 1), (1024, 128, 1, 2)]
        self.b_out = kb.buf("out")

        self.ones_f = self.sb("ones_f", [128, 128], F32)
        self.ident_f = self.sb("ident_f", [128, 128], F32)
        self.modT = self.sb("modT", [128, DEPTH, 96, 2], F32)
        self.ngs = self.sb("ngs", [128, DEPTH, 2, NCH], F32)
        self.ident_b = self.sb("ident_b", [128, 128], BF16)
        self.b_const = kb.buf("const")
        self.b_modT = kb.buf("modT")
        kb.op("dve", [], [self.b_const], lambda E: E.memset(self.ones_f[:], 1.0))
        kb.op("dve", [], [self.b_const], lambda E: E.memset(self.ident_f[:], 1.0))
        kb.op("pool", [self.b_const], [self.b_const], lambda E: E.affine_select(
            out=self.ident_f[:], in_=self.ident_f[:], pattern=[[-1, 128]], compare_op=ALU.is_equal,
            fill=0.0, base=0, channel_multiplier=1))
        kb.op("dve", [self.b_const], [self.b_const], lambda E: E.tensor_copy(out=self.ident_b[:], in_=self.ident_f[:]))
        kb.dma("sp", self.ngs[:], ngT[:, :, :, :], [], [self.b_const])
        self.psum = [nc.alloc_psum_tensor("ps%d" % i, [128, 512], F32) for i in range(8)]
        self.b_ps = [kb.buf("ps%d" % i) for i in range(8)]

        self.phase_mod(cT, w_mod, b_mod)
        if self.upto == "mod":
            kb.barrier()
            t = kb.dma("sp", out[:, 0, 0:768], self.modT[:].rearrange("p l a c -> p (l a c)"),
                       [self.b_modT], [self.b_out])
            kb.final_wait([t])
            return
        for (lo, n, col, g) in self.LG:
            kb.dma("sp", lambda: self.tk(hT_d, g), xT[:, :, lo:lo + n], [], [self.b_hT[g]])
        kb.barrier()
        self.declare_weights()
        if self.upto == "moe_test":
            self.phase_moe(0, True)
        elif self.upto == "odd_test":
            self.phase_odd(1, True)
        elif self.upto == "even_test":
            self.phase_even(0, True)
        else:
            for l in range(DEPTH):
                need_ctx = l < 2
                if l % 2 == 0:
                    self.phase_even(l, need_ctx)
                else:
                    self.phase_odd(l, need_ctx)
                self.phase_moe(l, need_ctx)
        kb.barrier()
        toks = [kb.dma("sp", out[:, :, :], lambda: self.tk(hT_d, "lat"), [self.b_hT[0], self.b_hT[1]], [self.b_out])]
        kb.final_wait(toks)

    def declare_weights(self):
        self.wr = self.din("wr", [DEPTH, D, 36])
        self.br = self.din("br", [DEPTH, 36])
        self.w_gate = self.din("moe_w_gate", [DEPTH, 32, D, 512])
        self.w_up = self.din("moe_w_up", [DEPTH, 32, D, 512])
        self.w_down = self.din("moe_w_down", [DEPTH, 32, 512, D])
        self.sel32 = self.din("sel32", [32, 32, 128], BF16)
        self.even_w_in = self.din("even_w_in", [2, D, 7200])
        self.even_w_out = self.din("even_w_out", [2, D, D])
        self.convT = self.din("convT", [2, 128, 16, 3])
        self.qkgT = self.din("qkgT", [2, 128, 2])
        self.gate_bT = self.din("gate_bT", [2, 8, 4])
        self.head_gain = self.din("head_gain", [2, 1024])
        self.biasx = self.din("biasx", [2, 8, 5, 128, 576])
        self.maskT = self.din("maskT", [2, 128, 128])
        self.sel8 = self.din("sel8", [8, 8, 128])
        self.Jm = self.din("Jm", [128, 128])
        self.QK_d = self.dshared("QK_d", [16, 128, NT], BF16)
        self.QR_d = self.dshared("QR_d", [16, 128, NT], F32)
        self.V_d = self.dshared("V_d", [NT, 3072], BF16)
        self.G_d = self.dshared("G_d", [NT, 32], F32)
        self.YC_d = self.dshared("YC_d", [16, 128, NT], BF16)
        self.odd_w_in = self.din("odd_w_in", [2, D, 8192])
        self.odd_w_out = self.din("odd_w_out", [2, 4096, D])
        self.odd_wsT = self.din("odd_wsT", [2, 128, 8, 128])
        self.odd_bs = self.din("odd_bs", [2, 1024])
        self.odd_vg = self.din("odd_vg", [2, 128, 32])

    def norm_tile(self, l, which, col, h_ap, n, out_ap, b_h, b_out, tmp, b_tmp, psb, f32_out=None, b_f32=None, chunk_cb=None):
        kb = self.kb
        ps = self.psum[psb]
        bps = self.b_ps[psb]
        sq, rstd, t1, AB = tmp["sq"], tmp["rstd"], tmp["t1"], tmp["AB"]
        sh = which * 3
        kb.op("dve", [self.b_modT, self.b_const], [b_tmp["AB"]], lambda E: E.scalar_tensor_tensor(
            out=AB[:, 0, :], in0=self.modT[:, l, (sh + 1) * 16:(sh + 2) * 16, col], scalar=1.0,
            in1=self.ngs[:, l, which, :], op0=ALU.add, op1=ALU.mult))
        for c in range(NCH):
            s = c % 2
            if c % 2 == 0:
                kb.op("act", [b_h], [b_tmp["sq%d" % s]], lambda E: E.activation(
                    out=sq[s][:, :n], in_=h_ap[:, c, :], func=AF.Square))
            else:
                kb.op("pool", [b_h], [b_tmp["sq%d" % s]], lambda E: E.tensor_tensor(
                    out=sq[s][:, :n], in0=h_ap[:, c, :], in1=h_ap[:, c, :], op=ALU.mult))
            self.mm(ps[:, :n], self.ones_f[:], sq[s][:, :n], c == 0, c == NCH - 1,
                    [self.b_const, b_tmp["sq%d" % s]], [bps])
        kb.op("act", [bps], [b_tmp["rstd"]], lambda E: E.activation(
            out=rstd[:, :n], in_=ps[:, :n], func=AF.Sqrt, bias=EPS, scale=1.0 / D))
        kb.op("dve", [b_tmp["rstd"]], [b_tmp["rstd"]], lambda E: E.reciprocal(out=rstd[:, :n], in_=rstd[:, :n]))
        for c in range(NCH):
            s = c % 2
            kb.op("dve", [b_h, b_tmp["rstd"], b_tmp["AB"]], [b_tmp["t1%d" % s]], lambda E: E.scalar_tensor_tensor(
                out=t1[s][:, :n], in0=h_ap[:, c, :], scalar=AB[:, 0, c:c + 1], in1=rstd[:, :n],
                op0=ALU.mult, op1=ALU.mult))
            if f32_out is None:
                kb.op("act", [b_tmp["t1%d" % s], self.b_modT], [b_out], lambda E: E.activation(
                    out=out_ap[:, c, :], in_=t1[s][:, :n], func=AF.Identity,
                    bias=self.modT[:, l, sh * 16 + c, col:col + 1], scale=1.0))
            else:
                kb.op("act", [b_tmp["t1%d" % s], self.b_modT], [b_f32[s]], lambda E: E.activation(
                    out=f32_out[s][:, :n], in_=t1[s][:, :n], func=AF.Identity,
                    bias=self.modT[:, l, sh * 16 + c, col:col + 1], scale=1.0))
                kb.op("pool", [b_f32[s]], [b_out], lambda E: E.tensor_copy(out=out_ap[:, c, :], in_=f32_out[s][:, :n]))
                chunk_cb(c, f32_out[s], b_f32[s])

    def norm_tmp(self, sbt):
        kb = self.kb
        tmp = {"sq": [sbt("n_sq0", [128, 512], F32), sbt("n_sq1", [128, 512], F32)],
               "rstd": sbt("n_rstd", [128, 512], F32),
               "t1": [sbt("n_t10", [128, 512], F32), sbt("n_t11", [128, 512], F32)],
               "AB": sbt("n_AB", [128, 2, NCH], F32)}
        b = {k: kb.buf() for k in ("sq0", "sq1", "rstd", "t10", "t11", "AB")}
        return tmp, b

    def phase_even(self, l, need_ctx):
        nc, kb = self.nc, self.kb
        from contextlib import ExitStack
        i = l // 2
        w_in = self.even_w_in[i].rearrange("(k p) f -> p k f", p=128)
        LG = self.LG
        b_qk, b_qr, b_vd, b_gd, b_yc = kb.buf(), kb.buf(), kb.buf(), kb.buf(), kb.buf()
        QK_d, QR_d, V_d, G_d, YC_d = self.QK_d, self.QR_d, self.V_d, self.G_d, self.YC_d
        with ExitStack() as es:
            def sbt(name, shape, dt):
                return es.enter_context(nc.sbuf_tensor(self.un(name), list(shape), dt))
            xm = sbt("e_xm", [128, NCH, NLOC], BF16)
            hg = sbt("e_h", [128, NCH, 512], F32)
            tmp, b_tmp = self.norm_tmp(sbt)
            b_xm = [kb.buf() for _ in range(3)]
            b_h = kb.buf()
            for (lo, n, col, g) in LG:
                kb.dma("sp", hg[:, :, :n], lambda: self.tk(self.hT_d, g), [self.b_hT[g]], [b_h])
                self.norm_tile(l, 0, col, hg[:, :, :n], n, xm[:, :, lo:lo + n], b_h, b_xm[g], tmp, b_tmp, 0)
            wf = [sbt("e_wf%d" % k, [128, NCH, 128], BF16) for k in range(2)]
            wt = [sbt("e_wt%d" % k, [128, NCH, 512], BF16) for k in range(2)]
            raw = [sbt("e_raw%d" % k, [128, NLOC], F32) for k in range(2)]
            ob = [sbt("e_ob%d" % k, [128, NLOC], BF16) for k in range(2)]
            stg = sbt("e_stg", [128, 9, 512], BF16)
            gst = sbt("e_gst", [128, 9, 32], F32)
            qg = sbt("e_qg", [128, 2], F32)
            b_wf, b_wt = [kb.buf(), kb.buf()], [kb.buf(), kb.buf()]
            b_raw, b_ob = [kb.buf(), kb.buf()], [kb.buf(), kb.buf()]
            b_stg, b_gst, b_c = kb.buf(), kb.buf(), kb.buf()
            kb.dma("sp", qg[:], self.qkgT[i], [], [b_c])
            kb.op("dve", [b_c], [b_c], lambda E: E.tensor_scalar(out=qg[:, 0:1], in0=qg[:, 0:1], scalar1=128.0 ** -0.5,
                                                                 scalar2=None, op0=ALU.mult))
            sq, rstd = tmp["sq"], tmp["rstd"]
            for cc in range(32):
                s_ = cc % 2
                c0 = cc * 128 if cc < 8 else (1024 + (cc - 8) * 128 if cc < 16 else (3072 + (cc - 16) * 128 if cc < 24 else 4096 + (cc - 24) * 128))
                kb.dma("pool", wf[s_][:], w_in[:, :, c0:c0 + 128], [], [b_wf[s_]])
                for (lo, n, col, g) in LG:
                    pb = 1 + g % 2
                    ps = self.psum[pb]
                    for k in range(NCH):
                        self.mm(ps[:, :n], wf[s_][:, k, :], xm[:, k, lo:lo + n], k == 0, k == NCH - 1, [b_wf[s_], b_xm[g]], [self.b_ps[pb]])
                    kb.op("act", [self.b_ps[pb]], [b_raw[s_]], lambda E: E.activation(out=raw[s_][:, lo:lo + n], in_=ps[:, :n], func=AF.Copy))
                if cc < 16:
                    which = 0 if cc < 8 else 1
                    for (lo, n, col, g) in LG:
                        kb.op("act", [b_raw[s_]], [b_tmp["sq0"]], lambda E: E.activation(out=sq[0][:, :n], in_=raw[s_][:, lo:lo + n], func=AF.Square))
                        self.mm(self.psum[3][:, :n], self.ones_f[:], sq[0][:, :n], True, True, [self.b_const, b_tmp["sq0"]], [self.b_ps[3]])
                        kb.op("act", [self.b_ps[3]], [b_tmp["rstd"]], lambda E: E.activation(
                            out=rstd[:, :n], in_=self.psum[3][:, :n], func=AF.Sqrt, bias=EPS, scale=1.0 / 128))
                        kb.op("dve", [b_tmp["rstd"]], [b_tmp["rstd"]], lambda E: E.reciprocal(out=rstd[:, :n], in_=rstd[:, :n]))
                        kb.op("dve", [b_raw[s_], b_tmp["rstd"], b_c], [b_ob[s_]], lambda E: E.scalar_tensor_tensor(
                            out=ob[s_][:, lo:lo + n], in0=raw[s_][:, lo:lo + n], scalar=qg[:, which:which + 1], in1=rstd[:, :n],
                            op0=ALU.mult, op1=ALU.mult))
                    kb.dma("sp", lambda: self.tk(QK_d[cc], "lat"), ob[s_][:, 0:1024], [b_ob[s_]], [b_qk])
                    kb.dma("sp", lambda: self.tk(QK_d[cc], 2), ob[s_][:, 1024:NLOC], [b_ob[s_]], [b_qk])
                else:
                    kb.dma("sp", lambda: self.tk(QR_d[cc - 16], "lat"), raw[s_][:, 0:1024], [b_raw[s_]], [b_qr])
                    kb.dma("sp", lambda: self.tk(QR_d[cc - 16], 2), raw[s_][:, 1024:NLOC], [b_raw[s_]], [b_qr])
            Vv = V_d.rearrange("(T p) c -> p T c", p=128)
            Gv = G_d.rearrange("(T p) c -> p T c", p=128)
            for ct in range(7):
                s_ = ct % 2
                c0 = [2048, 2560, 5120, 5632, 6144, 6656, 7168][ct]
                m = 512 if ct < 6 else 32
                kb.dma("pool", wt[s_][:, :, :m], w_in[:, :, c0:c0 + m], [], [b_wt[s_]])
                for T in range(9):
                    pb = 1 + T % 2
                    ps = self.psum[pb]
                    g = min(T // 4, 2)
                    for k in range(NCH):
                        self.mm(ps[:, :m], xm[:, k, T * 128:(T + 1) * 128], wt[s_][:, k, :m], k == 0, k == NCH - 1,
                                [b_xm[g], b_wt[s_]], [self.b_ps[pb]])
                    if ct < 4:
                        kb.op("act", [self.b_ps[pb]], [b_stg], lambda E: E.activation(out=stg[:, T, :], in_=ps[:, :512], func=AF.Copy))
                    elif ct < 6:
                        kb.op("act", [self.b_ps[pb]], [b_stg], lambda E: E.activation(out=stg[:, T, :], in_=ps[:, :512], func=AF.Sigmoid))
                    else:
                        kb.op("act", [self.b_ps[pb]], [b_gst], lambda E: E.activation(out=gst[:, T, :], in_=ps[:, :32], func=AF.Copy))
                if ct < 6:
                    kb.dma("sp", lambda: Vv[:, 0:16, ct * 512:(ct + 1) * 512].rearrange("p (s t) c -> p s t c", s=2)[:, bass.ts(self.slot, 1)],
                           stg[:, 0:8, :].rearrange("p (s t) c -> p s t c", s=1), [b_stg], [b_vd])
                    kb.dma("sp", lambda: Vv[:, 16:18, ct * 512:(ct + 1) * 512][:, bass.ts(self.slot, 1)], stg[:, 8:9, :], [b_stg], [b_vd])
                else:
                    kb.dma("sp", lambda: Gv[:, 0:16, :].rearrange("p (s t) c -> p s t c", s=2)[:, bass.ts(self.slot, 1)],
                           gst[:, 0:8, :].rearrange("p (s t) c -> p s t c", s=1), [b_gst], [b_gd])
                    kb.dma("sp", lambda: Gv[:, 16:18, :][:, bass.ts(self.slot, 1)], gst[:, 8:9, :], [b_gst], [b_gd])
        self.pbar()
        self.phase_na(l, i, need_ctx, b_qk, b_vd, b_yc)
        self.phase_mlstm(l, i, b_qr, b_vd, b_gd, b_yc)
        self.pbar()
        with ExitStack() as es:
            def sbt(name, shape, dt):
                return es.enter_context(nc.sbuf_tensor(self.un(name), list(shape), dt))
            yc = sbt("o_yc", [128, 16, NLOC], BF16)
            wo = [sbt("o_w%d" % k, [128, 16, 128], BF16) for k in range(2)]
            ht = [sbt("o_h%d" % k, [128, 512], F32) for k in range(2)]
            b_ycs, b_wo, b_ht = kb.buf(), [kb.buf(), kb.buf()], [kb.buf(), kb.buf()]
            for c in range(16):
                kb.dma("sp", yc[:, c, 0:1024], lambda: self.tk(YC_d[c], "lat"), [b_yc], [b_ycs])
                kb.dma("sp", yc[:, c, 1024:NLOC], lambda: self.tk(YC_d[c], 2), [b_yc], [b_ycs])
            w_out = self.even_w_out[i].rearrange("(k p) d -> p k d", p=128)
            it = 0
            for dc in range(NCH):
                s_ = dc % 2
                kb.dma("pool", wo[s_][:], w_out[:, :, dc * 128:(dc + 1) * 128], [], [b_wo[s_]])
                for (lo, n, col, g) in LG:
                    if g == 2 and not need_ctx:
                        continue
                    hs = it % 2
                    it += 1
                    kb.dma("sp", ht[hs][:, :n], lambda: self.tk(self.hT_d[:, dc, :], g), [self.b_hT[g]], [b_ht[hs]])
                    ps = self.psum[1 + hs]
                    for k in range(16):
                        self.mm(ps[:, :n], wo[s_][:, k, :], yc[:, k, lo:lo + n], k == 0, k == 15, [b_wo[s_], b_ycs], [self.b_ps[1 + hs]])
                    kb.op("dve", [self.b_ps[1 + hs], b_ht[hs], self.b_modT], [b_ht[hs]], lambda E: E.scalar_tensor_tensor(
                        out=ht[hs][:, :n], in0=ps[:, :n], scalar=self.modT[:, l, 2 * 16 + dc, col:col + 1], in1=ht[hs][:, :n],
                        op0=ALU.mult, op1=ALU.add))
                    kb.dma("sp", lambda: self.tk(self.hT_d[:, dc, :], g), ht[hs][:, :n], [b_ht[hs]], [self.b_hT[g]])
            kb.barrier()

    def phase_na(self, l, i, need_ctx, b_qk, b_vd, b_yc):
        nc, kb = self.nc, self.kb
        from contextlib import ExitStack
        with ExitStack() as es:
            def sbt(name, shape, dt):
                return es.enter_context(nc.sbuf_tensor(self.un(name), list(shape), dt))
            qT = sbt("a_q", [128, NT], BF16)
            kT = sbt("a_k", [128, NT], BF16)
            va = sbt("a_v", [128, 18, 128], BF16)
            bia = sbt("a_b", [128, 5, 576], F32)
            Sb = sbt("a_S", [128, 832], F32)
            Pn = sbt("a_P", [128, 832], BF16)
            PT = sbt("a_PT", [128, 7, 128], BF16)
            oTs = sbt("a_o", [128, NT], BF16)
            st = sbt("a_st", [128, 4], F32)
            b_q, b_k, b_v, b_b, b_S, b_P, b_PT, b_o, b_st = [kb.buf() for _ in range(9)]
            psTb = self.psum[3][:].bitcast(BF16)
            for hh in range(4):
                kb.dma("sp", qT[:], lambda: self.hd3(self.QK_d, 0, hh), [b_qk], [b_q])
                kb.dma("sp", kT[:], lambda: self.hd3(self.QK_d, 1, hh), [b_qk], [b_k])
                kb.dma("sp", va[:], lambda: self.vcols(0, hh), [b_vd], [b_v])
                kb.dma("sp", bia[:], lambda: self.biasx[i].rearrange("(s e) q p k -> s e q p k", s=2)[bass.ts(self.slot, 1), hh]
                       .rearrange("s q p k -> p (s q) k"), [], [b_b])
                if not need_ctx:
                    kb.op("dve", [], [b_o], lambda E: E.memset(oTs[:, NL:NT], 0.0))
                for T in range(18 if need_ctx else 16):
                    q = qT[:, T * 128:(T + 1) * 128]
                    psA, psB = self.psum[1], self.psum[2]
                    if T < 16:
                        ks = min(max(T - 2, 0), 12)
                        pat = {0: 0, 1: 1, 14: 3, 15: 4}.get(T, 2)
                        self.mm(psA[:, 0:512], q, kT[:, ks * 128:ks * 128 + 512], True, True, [b_q, b_k], [self.b_ps[1]])
                        self.mm(psB[:, 0:64], q, kT[:, ks * 128 + 512:ks * 128 + 576], True, True, [b_q, b_k], [self.b_ps[2]])
                        self.mm(psB[:, 64:320], q, kT[:, NL:NT], True, True, [b_q, b_k], [self.b_ps[2]])
                        kb.op("dve", [self.b_ps[1], b_b], [b_S], lambda E: E.tensor_tensor(
                            out=Sb[:, 0:512], in0=psA[:, 0:512], in1=bia[:, pat, 0:512], op=ALU.add))
                        kb.op("dve", [self.b_ps[2], b_b], [b_S], lambda E: E.tensor_tensor(
                            out=Sb[:, 512:576], in0=psB[:, 0:64], in1=bia[:, pat, 512:576], op=ALU.add))
                        kb.op("act", [self.b_ps[2]], [b_S], lambda E: E.activation(out=Sb[:, 576:832], in_=psB[:, 64:320], func=AF.Copy))
                        W = 832
                        blocks = [(ks + b, 128, b * 128) for b in range(4)] + [(ks + 4, 64, 512), (16, 128, 576), (17, 128, 704)]
                    else:
                        self.mm(psA[:, 0:256], q, kT[:, NL:NT], True, True, [b_q, b_k], [self.b_ps[1]])
                        kb.op("act", [self.b_ps[1]], [b_S], lambda E: E.activation(out=Sb[:, 0:256], in_=psA[:, 0:256], func=AF.Copy))
                        W = 256
                        blocks = [(16, 128, 0), (17, 128, 128)]
                    kb.op("dve", [b_S], [b_st], lambda E: E.tensor_reduce(out=st[:, 0:1], in_=Sb[:, :W], axis=AX.X, op=ALU.max))
                    kb.op("dve", [b_st], [b_st], lambda E: E.tensor_scalar(out=st[:, 1:2], in0=st[:, 0:1], scalar1=-1.0, scalar2=None, op0=ALU.mult))
                    kb.op("act", [b_S, b_st], [b_S], lambda E: E.activation(out=Sb[:, :W], in_=Sb[:, :W], func=AF.Exp, bias=st[:, 1:2], scale=1.0))
                    kb.op("dve", [b_S], [b_st], lambda E: E.tensor_reduce(out=st[:, 2:3], in_=Sb[:, :W], axis=AX.X, op=ALU.add))
                    kb.op("dve", [b_st], [b_st], lambda E: E.reciprocal(out=st[:, 3:4], in_=st[:, 2:3]))
                    kb.op("dve", [b_S, b_st], [b_P], lambda E: E.tensor_scalar(out=Pn[:, :W], in0=Sb[:, :W], scalar1=st[:, 3:4], scalar2=None, op0=ALU.mult))
                    for bi, (vt, nk, c0) in enumerate(blocks):
                        kb.op("pe", [b_P, self.b_const], [self.b_ps[3]], lambda E: E.transpose(
                            psTb[0:nk, bi * 128:(bi + 1) * 128], Pn[:, c0:c0 + nk], self.ident_b[:]))
                        kb.op("act", [self.b_ps[3]], [b_PT], lambda E: E.activation(
                            out=PT[0:nk, bi, :], in_=psTb[0:nk, bi * 128:(bi + 1) * 128], func=AF.Copy))
                    psO = self.psum[4]
                    for bi, (vt, nk, c0) in enumerate(blocks):
                        self.mm(psO[:, 0:128], va[0:nk, vt, :], PT[0:nk, bi, :], bi == 0, bi == len(blocks) - 1, [b_v, b_PT], [self.b_ps[4]])
                    kb.op("act", [self.b_ps[4]], [b_o], lambda E: E.activation(out=oTs[:, T * 128:(T + 1) * 128], in_=psO[:, 0:128], func=AF.Copy))
                kb.dma("sp", lambda: self.hd3(self.YC_d, 0, hh), oTs[:], [b_o], [b_yc])
            kb.barrier()

    def phase_mlstm(self, l, i, b_qr, b_vd, b_gd, b_yc):
        nc, kb = self.nc, self.kb
        from contextlib import ExitStack
        ORD = [[16, 17] + list(range(16)), [17, 16] + list(range(15, -1, -1))]
        with ExitStack() as es0:
            def sb0(name, shape, dt):
                return es0.enter_context(nc.sbuf_tensor(self.un(name), list(shape), dt))
            TOK = [sb0("l_tok%d" % d, [128, 18, 5, 4], F32) for d in range(2)]
            ROW = [[sb0("l_row%d%d" % (d, a), [4, NT], F32) for a in range(2)] for d in range(2)]
            FO = [sb0("l_fo%d" % d, [4, 36], F32) for d in range(2)]
            gst = sb0("l_gst", [128, 18, 4, 4], F32)
            gb = sb0("l_gb", [4, 4], F32)
            sel = sb0("l_sel", [4, 4, 128], F32)
            cwq = sb0("l_cwq", [128, 4, 3], F32)
            cwk = sb0("l_cwk", [128, 4, 3], F32)
            Jm = sb0("l_J", [128, 128], F32)
            mk = sb0("l_mk", [128, 2, 128], F32)
            hgb = sb0("l_hg", [128, 128], F32)
            b_tok, b_row, b_fo = [kb.buf(), kb.buf()], [kb.buf(), kb.buf()], [kb.buf(), kb.buf()]
            b_c = kb.buf()
            for T in range(18):
                kb.dma("sp", gst[:, T, :, :].rearrange("p y (s e) -> p y s e", s=1),
                       lambda: self.G_d[T * 128:(T + 1) * 128, :].rearrange("p (y s e) -> p y s e", y=4, s=2)[:, :, bass.ts(self.slot, 1), :],
                       [b_gd], [b_c])
            kb.dma("sp", gb[:], lambda: self.gate_bT[i].rearrange("(s e) y -> s e y", s=2)[bass.ts(self.slot, 1)].rearrange("s e y -> (s e) y"), [], [b_c])
            kb.dma("sp", sel[:], self.sel8[0:4, 0:4, :], [], [b_c])
            cv = self.convT[i].rearrange("p (w s e) k -> p w s e k", w=2, s=2)
            kb.dma("sp", cwq[:], lambda: cv[:, 0, bass.ts(self.slot, 1)].rearrange("p s e k -> p (s e) k"), [], [b_c])
            kb.dma("sp", cwk[:], lambda: cv[:, 1, bass.ts(self.slot, 1)].rearrange("p s e k -> p (s e) k"), [], [b_c])
            kb.dma("sp", Jm[:], self.Jm[:, :], [], [b_c])
            kb.dma("sp", mk[:], self.maskT.rearrange("a p t -> p a t"), [], [b_c])
            with ExitStack() as es:
                def sbt(name, shape, dt):
                    return es.enter_context(nc.sbuf_tensor(self.un(name), list(shape), dt))
                A = {k: sbt("l_" + k, [4, NT], F32) for k in ("IG", "LF", "CU", "B", "PM", "GX", "WL", "FL")}
                PE_ = sbt("l_pe", [4, 37], F32)
                t40 = sbt("l_t40", [128, 20], F32)
                bA = {k: kb.buf() for k in A}
                b_pe, b_t40 = kb.buf(), kb.buf()
                v3 = lambda ap: ap.rearrange("p (c s) -> p c s", s=64)
                for d in range(2):
                    flip = self.ident_f if d == 0 else Jm
                    for p in range(18):
                        T = ORD[d][p]
                        for gi, key in ((0, "IG"), (1, "LF")):
                            self.mm(self.psum[5][0:4, 0:128], gst[:, T, 2 * d + gi, :], flip[:], True, True, [b_c, self.b_const], [self.b_ps[5]])
                            kb.op("act", [self.b_ps[5], b_c], [bA[key]], lambda E: E.activation(
                                out=A[key][:, p * 128:(p + 1) * 128], in_=self.psum[5][0:4, 0:128], func=AF.Identity,
                                bias=gb[:, 2 * d + gi:2 * d + gi + 1], scale=1.0))
                    kb.op("act", [bA["LF"]], [bA["LF"]], lambda E: E.activation(out=A["LF"][:], in_=A["LF"][:], func=AF.Sigmoid))
                    kb.op("act", [bA["LF"]], [bA["LF"]], lambda E: E.activation(out=A["LF"][:], in_=A["LF"][:], func=AF.Ln))
                    kb.op("dve", [bA["LF"], self.b_const], [bA["CU"]], lambda E: E.tensor_tensor_scan(
                        out=A["CU"][:], data0=self.ones_f[0:4, 0:1].to_broadcast([4, NT]), data1=A["LF"][:], initial=0.0,
                        op0=ALU.mult, op1=ALU.add))
                    kb.op("dve", [bA["IG"], bA["CU"]], [bA["B"]], lambda E: E.tensor_tensor(out=A["B"][:], in0=A["IG"][:], in1=A["CU"][:], op=ALU.subtract))
                    kb.op("dve", [bA["B"]], [bA["PM"]], lambda E: E.tensor_tensor_scan(
                        out=A["PM"][:], data0=A["B"][:], data1=A["B"][:], initial=0.0, op0=ALU.max, op1=ALU.max))
                    kb.op("dve", [], [b_pe], lambda E: E.memset(PE_[:, 0:1], 0.0))
                    kb.op("dve", [bA["PM"]], [b_pe], lambda E: E.tensor_copy(out=PE_[:, 1:37], in_=v3(A["PM"][:])[:, :, 63]))
                    kb.op("dve", [b_pe, bA["PM"]], [bA["GX"]], lambda E: E.tensor_tensor(
                        out=v3(A["GX"][:]), in0=PE_[:, 0:36].unsqueeze(2).to_broadcast([4, 36, 64]), in1=v3(A["PM"][:]), op=ALU.subtract))
                    kb.op("act", [bA["GX"]], [bA["GX"]], lambda E: E.activation(out=A["GX"][:], in_=A["GX"][:], func=AF.Exp))
                    kb.op("dve", [b_pe, bA["B"]], [bA["WL"]], lambda E: E.tensor_tensor(
                        out=v3(A["WL"][:]), in0=v3(A["B"][:]), in1=PE_[:, 1:37].unsqueeze(2).to_broadcast([4, 36, 64]), op=ALU.subtract))
                    kb.op("act", [bA["WL"]], [bA["WL"]], lambda E: E.activation(out=A["WL"][:], in_=A["WL"][:], func=AF.Exp))
                    kb.op("dve", [b_pe], [b_fo[d]], lambda E: E.tensor_tensor(out=FO[d][:], in0=PE_[:, 0:36], in1=PE_[:, 1:37], op=ALU.subtract))
                    kb.op("act", [b_fo[d]], [b_fo[d]], lambda E: E.activation(out=FO[d][:], in_=FO[d][:], func=AF.Exp))
                    kb.op("dve", [bA["CU"], bA["PM"]], [bA["FL"]], lambda E: E.tensor_tensor(out=A["FL"][:], in0=A["CU"][:], in1=A["PM"][:], op=ALU.add))
                    kb.op("act", [bA["FL"]], [bA["FL"]], lambda E: E.activation(out=A["FL"][:], in_=A["FL"][:], func=AF.Exp, scale=-1.0))
                    kb.op("dve", [bA["PM"]], [bA["PM"]], lambda E: E.tensor_scalar(out=A["PM"][:], in0=A["PM"][:], scalar1=-1.0, scalar2=None, op0=ALU.mult))
                    for p in range(18):
                        T = ORD[d][p]
                        for a, key in enumerate(("B", "WL", "FL", "PM", "GX")):
                            self.mm(self.psum[5][:, a * 4:(a + 1) * 4], A[key][:, p * 128:(p + 1) * 128], self.ident_f[0:4, 0:4], True, True,
                                    [bA[key], self.b_const], [self.b_ps[5]])
                        kb.op("act", [self.b_ps[5]], [b_t40], lambda E: E.activation(out=t40[:], in_=self.psum[5][:, 0:20], func=AF.Copy))
                        self.mm(self.psum[6][:, 0:20], flip[:], t40[:], True, True, [b_c, self.b_const, b_t40], [self.b_ps[6]])
                        kb.op("act", [self.b_ps[6]], [b_tok[d]], lambda E: E.activation(
                            out=TOK[d][:, T, :, :].rearrange("p a b -> p (a b)"), in_=self.psum[6][:, 0:20], func=AF.Copy))
                        for a in range(2):
                            self.mm(self.psum[7][0:4, a * 128:(a + 1) * 128], TOK[d][:, T, 3 + a, :], self.ident_f[:], True, True,
                                    [b_tok[d], self.b_const], [self.b_ps[7]])
                            kb.op("act", [self.b_ps[7]], [b_row[d]], lambda E: E.activation(
                                out=ROW[d][a][:, T * 128:(T + 1) * 128], in_=self.psum[7][0:4, a * 128:(a + 1) * 128], func=AF.Copy))
                kb.barrier()
            with ExitStack() as es:
                def sbt(name, shape, dt):
                    return es.enter_context(nc.sbuf_tensor(self.un(name), list(shape), dt))
                qT = sbt("l_q", [128, NT], BF16)
                kT = sbt("l_k", [128, NT], BF16)
                Va = sbt("l_va", [128, 18, 132], BF16)
                so = sbt("l_so", [128, 18, 128], BF16)
                kw = [sbt("l_kw%d" % d, [128, 18, 128], BF16) for d in range(2)]
                qg = [sbt("l_qg%d" % d, [128, NT], BF16) for d in range(2)]
                hout = sbt("l_ho", [128, 18, 128], F32)
                ycs = sbt("l_yc", [128, NT], BF16)
                fob = [sbt("l_fob%d" % d, [128, 36], F32) for d in range(2)]
                Cst = [sbt("l_C%d" % d, [128, 132], F32) for d in range(2)]
                Cbf = [[sbt("l_Cb%d%d" % (d, k), [128, 132], BF16) for k in range(2)] for d in range(2)]
                Et = sbt("l_E", [128, 128], F32)
                wT = sbt("l_w", [128, 128], BF16)
                sm = sbt("l_sm", [128, 8], F32)
                yt = sbt("l_yt", [128, 128], F32)
                rq = sbt("l_rq", [128, NT], F32)
                cy = sbt("l_cy", [128, NT], F32)
                b_rq, b_cy = kb.buf(), kb.buf()
                b_q, b_k, b_va, b_so, b_ho, b_ycs, b_E, b_w, b_sm, b_yt = [kb.buf() for _ in range(10)]
                b_kw, b_qg, b_fob, b_C = [kb.buf(), kb.buf()], [kb.buf(), kb.buf()], [kb.buf(), kb.buf()], [kb.buf(), kb.buf()]
                b_Cb = [[kb.buf(), kb.buf()], [kb.buf(), kb.buf()]]
                psKb = self.psum[5][:].bitcast(BF16)
                for h in range(4):
                    for qi, (cwt, dst, b_dst) in enumerate(((cwq, qT, b_q), (cwk, kT, b_k))):
                        kb.dma("sp", rq[:], lambda: self.hd3(self.QR_d, qi, h), [b_qr], [b_rq])
                        kb.op("dve", [b_rq, b_c], [b_cy], lambda E: E.tensor_scalar(out=cy[:], in0=rq[:], scalar1=cwt[:, h, 1:2],
                                                                                  scalar2=None, op0=ALU.mult))
                        for (a_, b_) in ((0, NL), (NL, NT)):
                            kb.op("dve", [b_rq, b_c, b_cy], [b_cy], lambda E: E.scalar_tensor_tensor(
                                out=cy[:, a_ + 1:b_], in0=rq[:, a_:b_ - 1], scalar=cwt[:, h, 0:1], in1=cy[:, a_ + 1:b_], op0=ALU.mult, op1=ALU.add))
                            kb.op("dve", [b_rq, b_c, b_cy], [b_cy], lambda E: E.scalar_tensor_tensor(
                                out=cy[:, a_:b_ - 1], in0=rq[:, a_ + 1:b_], scalar=cwt[:, h, 2:3], in1=cy[:, a_:b_ - 1], op0=ALU.mult, op1=ALU.add))
                        if qi == 0:
                            kb.op("act", [b_cy], [b_dst], lambda E: E.activation(out=dst[:], in_=cy[:], func=AF.Silu))
                        else:
                            kb.op("act", [b_cy], [b_cy], lambda E: E.activation(out=cy[:], in_=cy[:], func=AF.Silu))
                            kb.op("pool", [b_cy], [b_dst], lambda E: E.tensor_scalar(out=dst[:], in0=cy[:], scalar1=128.0 ** -0.5,
                                                                                   scalar2=None, op0=ALU.mult))
                    kb.dma("sp", Va[:, :, 0:128], lambda: self.vcols(1, h), [b_vd], [b_va])
                    kb.op("dve", [b_va], [b_va], lambda E: E.memset(Va[:, :, 128:129], 1.0))
                    kb.dma("sp", so[:], lambda: self.vcols(2, h), [b_vd], [b_so])
                    kb.dma("sp", hgb[:, 0:128], lambda: self.head_gain[i:i + 1, :].rearrange("o (s e j) -> o s e j", s=2, e=4)[:, bass.ts(self.slot, 1), h, :]
                           .rearrange("o s j -> o (s j)").partition_broadcast(128), [], [b_c])
                    for d in range(2):
                        self.mm(self.psum[6][:, 0:36], sel[:, h, :], FO[d][:], True, True, [b_c, b_fo[d]], [self.b_ps[6]])
                        kb.op("act", [self.b_ps[6]], [b_fob[d]], lambda E: E.activation(out=fob[d][:], in_=self.psum[6][:, 0:36], func=AF.Copy))
                        for (t0, n) in ((0, 512), (512, 512), (1024, 512), (1536, 512), (2048, 256)):
                            self.mm(self.psum[7][:, :n], sel[:, h, :], ROW[d][1][:, t0:t0 + n], True, True, [b_c, b_row[d]], [self.b_ps[7]])
                            kb.op("dve", [self.b_ps[7], b_q], [b_qg[d]], lambda E: E.tensor_tensor(
                                out=qg[d][:, t0:t0 + n], in0=self.psum[7][:, :n], in1=qT[:, t0:t0 + n], op=ALU.mult))
                        kb.op("dve", [], [b_C[d]], lambda E: E.memset(Cst[d][:], 0.0))
                        kb.op("dve", [], [b_Cb[d][0]], lambda E: E.memset(Cbf[d][0][:], 0.0))
                    for T in range(18):
                        kb.op("pe", [b_k, self.b_const], [self.b_ps[5]], lambda E: E.transpose(
                            psKb[:, 0:128], kT[:, T * 128:(T + 1) * 128], self.ident_b[:]))
                        for d in range(2):
                            kb.op("dve", [self.b_ps[5], b_tok[d]], [b_kw[d]], lambda E: E.tensor_scalar(
                                out=kw[d][:, T, :], in0=psKb[:, 0:128], scalar1=TOK[d][:, T, 1, h:h + 1], scalar2=None, op0=ALU.mult))
                    for p in range(18):
                        for d in range(2):
                            T = ORD[d][p]
                            halves = (0, 1) if d == 0 else (1, 0)
                            first = (d == 0) == ((T + 2 if T < 16 else T - 16) <= (17 - T if T < 16 else 17 - T))
                            jF = T + 2 if T < 16 else T - 16
                            jR = 17 - T
                            first = (jF <= jR) if d == 0 else (jR < jF)
                            for hi, hf in enumerate(halves):
                                ch = 2 * p + hi
                                r0 = hf * 64
                                self.mm(self.psum[4][:, 0:129], kw[d][r0:r0 + 64, T, :], Va[r0:r0 + 64, T, 0:129], True, True,
                                        [b_kw[d], b_va], [self.b_ps[4]])
                                kb.op("dve", [self.b_ps[4], b_fob[d], b_C[d]], [b_C[d]], lambda E: E.scalar_tensor_tensor(
                                    out=Cst[d][:, 0:129], in0=Cst[d][:, 0:129], scalar=fob[d][:, ch:ch + 1], in1=self.psum[4][:, 0:129],
                                    op0=ALU.mult, op1=ALU.add))
                                if hi == 0:
                                    kb.op("act", [b_C[d]], [b_Cb[d][1]], lambda E: E.activation(
                                        out=Cbf[d][1][:, 0:129], in_=Cst[d][:, 0:129], func=AF.Copy))
                                if hi == 0:
                                    tc = slice(T * 128, (T + 1) * 128)
                                    self.mm(self.psum[1][:, 0:128], kT[:, tc], qT[:, tc], True, True, [b_k, b_q], [self.b_ps[1]])
                                    self.mm(self.psum[2][:, 0:128], sel[:, h, :], ROW[d][0][:, tc], True, False, [b_c, b_row[d]], [self.b_ps[2]])
                                    self.mm(self.psum[2][:, 0:128], self.ident_f[:], mk[:, d, :], False, True, [b_c, self.b_const], [self.b_ps[2]])
                                    kb.op("act", [self.b_ps[2], b_tok[d]], [b_E], lambda E: E.activation(
                                        out=Et[:], in_=self.psum[2][:, 0:128], func=AF.Exp, bias=TOK[d][:, T, 0, h:h + 1], scale=1.0))
                                    kb.op("dve", [self.b_ps[1], b_E], [b_w], lambda E: E.tensor_tensor(
                                        out=wT[:], in0=self.psum[1][:, 0:128], in1=Et[:], op=ALU.mult))
                                    self.mm(self.psum[3][:, 0:129], wT[:], Va[:, T, 0:129], True, False, [b_w, b_va], [self.b_ps[3]])
                            for hi, hf in enumerate(halves):
                                r0 = hf * 64
                                self.mm(self.psum[3][r0:r0 + 64, 0:129], qg[d][:, T * 128 + r0:T * 128 + r0 + 64], Cbf[d][hi][:, 0:129],
                                        False, hi == 1, [b_qg[d], b_Cb[d][hi]], [self.b_ps[3]])
                            kb.op("act", [self.b_ps[3]], [b_sm], lambda E: E.activation(out=sm[:, 0:1], in_=self.psum[3][:, 128:129], func=AF.Abs))
                            kb.op("dve", [b_sm, b_tok[d]], [b_sm], lambda E: E.tensor_tensor(out=sm[:, 1:2], in0=sm[:, 0:1], in1=TOK[d][:, T, 2, h:h + 1], op=ALU.max))
                            kb.op("dve", [b_sm], [b_sm], lambda E: E.reciprocal(out=sm[:, 2:3], in_=sm[:, 1:2]))
                            if first:
                                kb.op("dve", [self.b_ps[3], b_sm], [b_ho], lambda E: E.tensor_scalar(
                                    out=hout[:, T, :], in0=self.psum[3][:, 0:128], scalar1=sm[:, 2:3], scalar2=None, op0=ALU.mult))
                            else:
                                kb.op("dve", [self.b_ps[3], b_sm, b_ho], [b_ho], lambda E: E.scalar_tensor_tensor(
                                    out=hout[:, T, :], in0=self.psum[3][:, 0:128], scalar=sm[:, 2:3], in1=hout[:, T, :], op0=ALU.mult, op1=ALU.add))
                            kb.op("act", [b_C[d]], [b_Cb[d][0]], lambda E: E.activation(out=Cbf[d][0][:, 0:129], in_=Cst[d][:, 0:129], func=AF.Copy))
                    for T in range(18):
                        kb.op("act", [b_ho], [b_yt, b_sm], lambda E: E.activation(out=yt[:], in_=hout[:, T, :], func=AF.Square, accum_out=sm[:, 4:5]))
                        kb.op("act", [b_sm], [b_sm], lambda E: E.activation(out=sm[:, 5:6], in_=sm[:, 4:5], func=AF.Sqrt, bias=EPS, scale=1.0 / 128))
                        kb.op("dve", [b_sm], [b_sm], lambda E: E.reciprocal(out=sm[:, 6:7], in_=sm[:, 5:6]))
                        kb.op("dve", [b_ho, b_sm, b_c], [b_yt], lambda E: E.scalar_tensor_tensor(
                            out=yt[:], in0=hout[:, T, :], scalar=sm[:, 6:7], in1=hgb[:, 0:128], op0=ALU.mult, op1=ALU.mult))
                        kb.op("dve", [b_yt, b_so], [b_yt], lambda E: E.tensor_tensor(out=yt[:], in0=yt[:], in1=so[:, T, :], op=ALU.mult))
                        self.mm(self.psum[6][:, 0:128], yt[:], self.ident_f[:], True, True, [b_yt, self.b_const], [self.b_ps[6]])
                        kb.op("act", [self.b_ps[6]], [b_ycs], lambda E: E.activation(out=ycs[:, T * 128:(T + 1) * 128], in_=self.psum[6][:, 0:128], func=AF.Copy))
                    kb.dma("sp", lambda: self.hd3(self.YC_d, 1, h), ycs[:], [b_ycs], [b_yc])
                kb.barrier()

    def phase_odd(self, l, need_ctx):
        nc, kb = self.nc, self.kb
        from contextlib import ExitStack
        i = l // 2
        groups = [self.LG[0], self.LG[1]] + ([self.LG[2]] if need_ctx else [])
        w_in, w_out = self.odd_w_in[i], self.odd_w_out[i]
        for (t0, n, col, g) in groups:
            nT = n // 128
            with ExitStack() as es:
                def sbt(name, shape, dt):
                    return es.enter_context(nc.sbuf_tensor(self.un(name), list(shape), dt))
                hg = sbt("s_h", [128, NCH, 512], F32)
                xm = sbt("s_xm", [128, NCH, 512], BF16)
                vt = sbt("s_vt", [128, 4, 4096], BF16)
                pr = sbt("s_pr", [128, 32, 512], BF16)
                wv = [sbt("s_wv%d" % k, [128, NCH, 256], BF16) for k in range(2)]
                wu = [sbt("s_wu%d" % k, [128, NCH, 128], BF16) for k in range(2)]
                wo = [sbt("s_wo%d" % k, [128, 32, 128], BF16) for k in range(2)]
                gt = [sbt("s_g%d" % k, [128, 512], F32) for k in range(4)]
                wsT = sbt("s_wsT", [128, 8, 128], F32)
                wsc = sbt("s_wsc", [128, 4, 8, 128], BF16)
                bbc = sbt("s_bbc", [128, 8, 128], F32)
                vg = sbt("s_vg", [128, 32], F32)
                ss = sbt("s_ss", [128, 4, 16], F32)
                rs = sbt("s_rs", [128, 8], F32)
                tmp, b_tmp = self.norm_tmp(sbt)
                b_h, b_xm, b_vt, b_pr, b_c, b_ss, b_rs, b_wsc = [kb.buf() for _ in range(8)]
                b_wv, b_wu, b_wo = [kb.buf(), kb.buf()], [kb.buf(), kb.buf()], [kb.buf(), kb.buf()]
                b_g = [kb.buf() for _ in range(4)]
                kb.dma("sp", wsT[:], self.odd_wsT[i], [], [b_c])
                kb.dma("sp", bbc[:].rearrange("p a b -> p (a b)"), self.odd_bs[i:i + 1, :].partition_broadcast(128), [], [b_c])
                kb.dma("sp", vg[:], self.odd_vg[i], [], [b_c])
                kb.dma("sp", hg[:, :, :n], lambda: self.tk(self.hT_d, g), [self.b_hT[g]], [b_h])
                kb.op("dve", [], [b_ss], lambda E: E.memset(ss[:], 0.0))
                self.norm_tile(l, 0, col, hg[:, :, :n], n, xm[:, :, :n], b_h, b_xm, tmp, b_tmp, 0)

                def gelu(ps_ap, bps, m, out_f32_idx):
                    x, a, r = gt[0], gt[1], gt[out_f32_idx]
                    kb.op("act", [bps], [b_g[0]], lambda E: E.activation(out=x[:, :m], in_=ps_ap, func=AF.Copy))
                    kb.op("pool", [b_g[0]], [b_g[1]], lambda E: E.tensor_tensor(out=a[:, :m], in0=x[:, :m], in1=x[:, :m], op=ALU.mult))
                    kb.op("dve", [b_g[1]], [b_g[1]], lambda E: E.tensor_scalar(out=a[:, :m], in0=a[:, :m], scalar1=0.044715,
                                                                              scalar2=1.0, op0=ALU.mult, op1=ALU.add))
                    kb.op("pool", [b_g[0], b_g[1]], [b_g[1]], lambda E: E.tensor_tensor(out=a[:, :m], in0=a[:, :m], in1=x[:, :m], op=ALU.mult))
                    kb.op("act", [b_g[1]], [b_g[1]], lambda E: E.activation(out=a[:, :m], in_=a[:, :m], func=AF.Sigmoid,
                                                                           scale=1.5957691216057308))
                    kb.op("dve", [b_g[0], b_g[1]], [b_g[out_f32_idx]], lambda E: E.tensor_tensor(
                        out=r[:, :m], in0=x[:, :m], in1=a[:, :m], op=ALU.mult))
                for ct in range(16):
                    s_ = ct % 2
                    c0 = 4096 + ct * 256
                    kb.dma("pool", wv[s_][:], w_in.rearrange("(k p) f -> p k f", p=128)[:, :, c0:c0 + 256], [], [b_wv[s_]])
                    for T in range(nT):
                        pb = 1 + (ct * nT + T) % 2
                        ps = self.psum[pb]
                        for k in range(NCH):
                            self.mm(ps[:, 0:256], xm[:, k, T * 128:(T + 1) * 128], wv[s_][:, k, :], k == 0, k == NCH - 1,
                                    [b_xm, b_wv[s_]], [self.b_ps[pb]])
                        gelu(ps[:, 0:256], self.b_ps[pb], 256, 2)
                        kb.op("act", [b_g[2]], [b_g[1], b_ss], lambda E: E.activation(
                            out=gt[1][:, :256], in_=gt[2][:, :256], func=AF.Square, accum_out=ss[:, T, ct:ct + 1]))
                        kb.op("pool", [b_g[2]], [b_vt], lambda E: E.tensor_copy(
                            out=vt[:, T, ct * 256:(ct + 1) * 256], in_=gt[2][:, :256]))
                kb.op("dve", [b_ss], [b_rs], lambda E: E.tensor_reduce(out=rs[:, 0:4], in_=ss[:], axis=AX.X, op=ALU.add))
                kb.op("act", [b_rs], [b_rs], lambda E: E.activation(out=rs[:, 0:4], in_=rs[:, 0:4], func=AF.Sqrt,
                                                                    bias=EPS, scale=1.0 / 4096))
                kb.op("dve", [b_rs], [b_rs], lambda E: E.reciprocal(out=rs[:, 4:8], in_=rs[:, 0:4]))
                for T in range(nT):
                    kb.op("dve", [b_rs, b_c], [b_wsc], lambda E: E.tensor_scalar(
                        out=wsc[:, T, :, :], in0=wsT[:], scalar1=rs[:, 4 + T:5 + T], scalar2=None, op0=ALU.mult))
                for j in range(32):
                    s_ = j % 2
                    gi = j // 4
                    kb.dma("pool", wu[s_][:], w_in.rearrange("(k p) f -> p k f", p=128)[:, :, j * 128:(j + 1) * 128], [], [b_wu[s_]])
                    psU = self.psum[3 + s_]
                    for k in range(NCH):
                        self.mm(psU[:, :n], wu[s_][:, k, :], xm[:, k, :n], k == 0, k == NCH - 1, [b_wu[s_], b_xm], [self.b_ps[3 + s_]])
                    gelu(psU[:, :n], self.b_ps[3 + s_], n, 2)
                    psS = self.psum[5 + s_]
                    for T in range(nT):
                        self.mm(psS[:, T * 128:(T + 1) * 128], vt[:, T, j * 128:(j + 1) * 128], wsc[:, T, gi, :], True, True,
                                [b_vt, b_wsc], [self.b_ps[5 + s_]])
                    kb.op("dve", [self.b_ps[5 + s_], b_c], [b_g[3]], lambda E: E.scalar_tensor_tensor(
                        out=gt[3][:, :n].rearrange("p (a b) -> p a b", b=128), in0=psS[:, :n].rearrange("p (a b) -> p a b", b=128),
                        scalar=vg[:, j:j + 1], in1=bbc[:, gi:gi + 1, :].to_broadcast([128, nT, 128]),
                        op0=ALU.mult, op1=ALU.add))
                    kb.op("pool", [b_g[3], b_g[2]], [b_pr], lambda E: E.tensor_tensor(
                        out=pr[:, j, :n], in0=gt[3][:, :n], in1=gt[2][:, :n], op=ALU.mult))
                for dc in range(NCH):
                    s_ = dc % 2
                    kb.dma("pool", wo[s_][:], w_out.rearrange("(k p) d -> p k d", p=128)[:, :, dc * 128:(dc + 1) * 128], [], [b_wo[s_]])
                    psO = self.psum[1 + s_]
                    for k in range(32):
                        self.mm(psO[:, :n], wo[s_][:, k, :], pr[:, k, :n], k == 0, k == 31, [b_wo[s_], b_pr], [self.b_ps[1 + s_]])
                    kb.op("dve", [self.b_ps[1 + s_], b_h, self.b_modT], [b_h], lambda E: E.scalar_tensor_tensor(
                        out=hg[:, dc, :n], in0=psO[:, :n], scalar=self.modT[:, l, 2 * 16 + dc, col:col + 1],
                        in1=hg[:, dc, :n], op0=ALU.mult, op1=ALU.add))
                kb.dma("sp", lambda: self.tk(self.hT_d, g), hg[:, :, :n], [b_h], [self.b_hT[g]])
                kb.barrier()

    def phase_moe(self, l, need_ctx):
        nc, kb = self.nc, self.kb
        from contextlib import ExitStack
        for p in range(1):
            with ExitStack() as es:
                def sbt(name, shape, dt):
                    return es.enter_context(nc.sbuf_tensor(self.un(name), list(shape), dt))
                subs = [self.LG[0], self.LG[1]] + ([self.LG[2]] if need_ctx else [])
                hh = sbt("m_h", [128, NCH, 1152], F32)
                xm2 = sbt("m_xm2", [128, NCH, 1152], BF16)
                wr_s = sbt("m_wr", [128, NCH, 36], F32)
                br_s = sbt("m_br", [1, 36], F32)
                sel_s = sbt("m_sel", [32, 32, 128], BF16)
                combT = sbt("m_combT", [32, 1152], BF16)
                rt = sbt("m_rt", [128, 8, 40], F32)
                wg = [sbt("m_wg%d" % i, [128, NCH, 256], BF16) for i in range(2)]
                wu = [sbt("m_wu%d" % i, [128, NCH, 256], BF16) for i in range(2)]
                wd = [sbt("m_wd%d" % i, [128, 2, D], BF16) for i in range(2)]
                t1 = [sbt("m_t1%d" % i, [128, 512], F32) for i in range(2)]
                hid0 = [sbt("m_hid%d" % f, [128, 512], BF16) for f in range(2)]
                hid = [hid0, hid0]
                tmp, b_tmp = self.norm_tmp(sbt)
                t2 = tmp["t1"]
                xf = t1
                b_hh = [kb.buf() for _ in subs]
                b_xm = [kb.buf() for _ in subs]
                b_wr, b_sel, b_comb, b_rt = kb.buf(), kb.buf(), kb.buf(), kb.buf()
                b_wg, b_wu, b_wd = [kb.buf(), kb.buf()], [kb.buf(), kb.buf()], [kb.buf(), kb.buf()]
                b_t1, b_t2 = [kb.buf(), kb.buf()], [b_tmp["t10"], b_tmp["t11"]]
                b_xf = b_t1
                bh0 = [kb.buf(), kb.buf()]
                b_hid = [bh0, bh0]
                kb.dma("sp", wr_s[:], self.wr[l].rearrange("(k p) n -> p k n", p=128), [], [b_wr])
                kb.dma("sp", br_s[:], self.br[l:l + 1, :], [], [b_wr])
                kb.dma("sp", sel_s[:], self.sel32[:, :, :], [], [b_sel])
                offs = []
                o = 0
                for (t0, n, col, g) in subs:
                    offs.append(o)
                    o += n
                for si, (t0, n, col, g) in enumerate(subs):
                    o = offs[si]
                    kb.dma("sp", hh[:, :, o:o + n], lambda: self.tk(self.hT_d, g), [self.b_hT[g]], [b_hh[si]])
                    nj = n // 128

                    def cb(c, xf_t, b_xf_t, o=o, nj=nj):
                        for j in range(nj):
                            self.mm(self.psum[4 + j][:, 0:36], xf_t[:, j * 128:(j + 1) * 128], wr_s[:, c, :],
                                    c == 0, False, [b_xf_t, b_wr], [self.b_ps[4 + j]])
                    self.norm_tile(l, 1, col, hh[:, :, o:o + n], n, xm2[:, :, o:o + n], b_hh[si], b_xm[si],
                                   tmp, b_tmp, 0, f32_out=xf, b_f32=b_xf, chunk_cb=cb)
                    for j in range(nj):
                        psr = self.psum[4 + j]
                        self.mm(psr[:, 0:36], self.ones_f[0:1, 0:128], br_s[0:1, :], False, True,
                                [self.b_const, b_wr], [self.b_ps[4 + j]])
                        L = rt[:, 0, 0:36]
                        R = lambda k, a, b: rt[:, k, a:b]
                        V = lambda reads, fn: kb.op("dve", reads, [b_rt], fn)
                        V([self.b_ps[4 + j]], lambda E: E.tensor_copy(out=L, in_=psr[:, 0:36]))
                        V([b_rt], lambda E: E.tensor_reduce(out=R(1, 0, 1), in_=rt[:, 0, 0:4], axis=AX.X, op=ALU.max))
                        V([b_rt], lambda E: E.tensor_scalar(out=R(1, 4, 8), in0=rt[:, 0, 0:4], scalar1=R(1, 0, 1),
                                                            scalar2=None, op0=ALU.is_equal))
                        V([b_rt], lambda E: E.tensor_scalar(out=R(1, 1, 2), in0=R(1, 0, 1), scalar1=-1.0,
                                                            scalar2=None, op0=ALU.mult))
                        kb.op("act", [b_rt], [b_rt], lambda E: E.activation(out=R(1, 8, 12), in_=rt[:, 0, 0:4],
                                                                            func=AF.Exp, bias=R(1, 1, 2), scale=1.0))
                        V([b_rt], lambda E: E.tensor_reduce(out=R(1, 2, 3), in_=R(1, 8, 12), axis=AX.X, op=ALU.add))
                        V([b_rt], lambda E: E.tensor_scalar(out=R(1, 12, 16), in0=R(1, 4, 8), scalar1=-1.0, scalar2=1e30,
                                                            op0=ALU.add, op1=ALU.mult))
                        V([b_rt], lambda E: E.tensor_tensor(
                            out=rt[:, 2, 0:32].rearrange("p (g e) -> p g e", e=8),
                            in0=rt[:, 0, 4:36].rearrange("p (g e) -> p g e", e=8),
                            in1=R(1, 12, 16).unsqueeze(2).to_broadcast([128, 4, 8]), op=ALU.add))
                        V([b_rt], lambda E: E.tensor_reduce(out=R(1, 16, 17), in_=rt[:, 2, 0:32], axis=AX.X, op=ALU.max))
                        V([b_rt], lambda E: E.tensor_scalar(out=rt[:, 3, 0:32], in0=rt[:, 2, 0:32], scalar1=R(1, 16, 17),
                                                            scalar2=-1e30, op0=ALU.is_equal, op1=ALU.mult))
                        V([b_rt], lambda E: E.tensor_tensor(out=rt[:, 3, 0:32], in0=rt[:, 3, 0:32], in1=rt[:, 2, 0:32],
                                                            op=ALU.add))
                        V([b_rt], lambda E: E.tensor_reduce(out=R(1, 17, 18), in_=rt[:, 3, 0:32], axis=AX.X, op=ALU.max))
                        V([b_rt], lambda E: E.tensor_scalar(out=rt[:, 4, 0:32], in0=rt[:, 2, 0:32], scalar1=R(1, 17, 18),
                                                            scalar2=None, op0=ALU.is_ge))
                        V([b_rt], lambda E: E.tensor_scalar(out=R(1, 18, 19), in0=R(1, 16, 17), scalar1=-1.0,
                                                            scalar2=None, op0=ALU.mult))
                        kb.op("act", [b_rt], [b_rt], lambda E: E.activation(out=rt[:, 5, 0:32], in_=rt[:, 2, 0:32],
                                                                            func=AF.Exp, bias=R(1, 18, 19), scale=1.0))
                        V([b_rt], lambda E: E.tensor_tensor(out=rt[:, 5, 0:32], in0=rt[:, 5, 0:32], in1=rt[:, 4, 0:32],
                                                            op=ALU.mult))
                        V([b_rt], lambda E: E.tensor_reduce(out=R(1, 19, 20), in_=rt[:, 5, 0:32], axis=AX.X, op=ALU.add))
                        V([b_rt], lambda E: E.tensor_tensor(out=R(1, 20, 21), in0=R(1, 19, 20), in1=R(1, 2, 3), op=ALU.mult))
                        V([b_rt], lambda E: E.reciprocal(out=R(1, 21, 22), in_=R(1, 20, 21)))
                        V([b_rt], lambda E: E.tensor_scalar(out=rt[:, 6, 0:32], in0=rt[:, 5, 0:32], scalar1=R(1, 21, 22),
                                                            scalar2=None, op0=ALU.mult))
                        self.mm(self.psum[1][0:32, 0:128], rt[:, 6, 0:32], self.ident_f[:], True, True,
                                [b_rt, self.b_const], [self.b_ps[1]])
                        kb.op("act", [self.b_ps[1]], [b_comb], lambda E: E.activation(
                            out=combT[:, o + j * 128:o + (j + 1) * 128], in_=self.psum[1][0:32, 0:128], func=AF.Copy))
                it = 0
                for e in range(32):
                    for hf in range(2):
                        s = it % 2
                        it += 1
                        f0 = hf * 256
                        kb.dma("pool", wg[s][:], self.w_gate[l, e].rearrange("(k p) f -> p k f", p=128)[:, :, f0:f0 + 256],
                               [], [b_wg[s]])
                        kb.dma("pool", wu[s][:], self.w_up[l, e].rearrange("(k p) f -> p k f", p=128)[:, :, f0:f0 + 256],
                               [], [b_wu[s]])
                        kb.dma("pool", wd[s][:], self.w_down[l, e, f0:f0 + 256, :].rearrange("(k p) d -> p k d", p=128),
                               [], [b_wd[s]])
                        for si, (t0, n, col, g) in enumerate(subs):
                            o = offs[si]
                            hs = si % 2
                            psC = self.psum[4]
                            self.mm(psC[:, :n], sel_s[:, e, :], combT[:, o:o + n], True, True, [b_sel, b_comb], [self.b_ps[4]])
                            for fc in range(2):
                                psG, psU = self.psum[fc], self.psum[2 + fc]
                                for k in range(NCH):
                                    self.mm(psG[:, :n], wg[s][:, k, fc * 128:(fc + 1) * 128], xm2[:, k, o:o + n],
                                            k == 0, k == NCH - 1, [b_wg[s], b_xm[si]], [self.b_ps[fc]])
                                for k in range(NCH):
                                    self.mm(psU[:, :n], wu[s][:, k, fc * 128:(fc + 1) * 128], xm2[:, k, o:o + n],
                                            k == 0, k == NCH - 1, [b_wu[s], b_xm[si]], [self.b_ps[2 + fc]])
                                kb.op("act", [self.b_ps[fc]], [b_t1[fc]], lambda E: E.activation(
                                    out=t1[fc][:, :n], in_=psG[:, :n], func=AF.Silu))
                                kb.op("dve", [self.b_ps[2 + fc], b_t1[fc]], [b_t2[fc]], lambda E: E.tensor_tensor(
                                    out=t2[fc][:, :n], in0=psU[:, :n], in1=t1[fc][:, :n], op=ALU.mult))
                                kb.op("dve", [self.b_ps[4], b_t2[fc]], [b_hid[hs][fc]], lambda E: E.tensor_tensor(
                                    out=hid[hs][fc][:, :n], in0=psC[:, :n], in1=t2[fc][:, :n], op=ALU.mult))
                            for dc in range(NCH):
                                pb = 5 + dc % 3
                                psO = self.psum[pb]
                                for fc in range(2):
                                    self.mm(psO[:, :n], wd[s][:, fc, dc * 128:(dc + 1) * 128], hid[hs][fc][:, :n],
                                            fc == 0, fc == 1, [b_wd[s], b_hid[hs][fc]], [self.b_ps[pb]])
                                kb.op("dve", [self.b_ps[pb], b_hh[si], self.b_modT], [b_hh[si]],
                                      lambda E: E.scalar_tensor_tensor(
                                          out=hh[:, dc, o:o + n], in0=psO[:, :n], scalar=self.modT[:, l, 5 * 16 + dc, col:col + 1],
                                          in1=hh[:, dc, o:o + n], op0=ALU.mult, op1=ALU.add))
                for si, (t0, n, col, g) in enumerate(subs):
                    o = offs[si]
                    kb.dma("sp", lambda: self.tk(self.hT_d, g), hh[:, :, o:o + n], [b_hh[si]], [self.b_hT[g]])
                kb.barrier()

    def phase_mod(self, cT, w_mod, b_mod):
        nc, kb = self.nc, self.kb
        from contextlib import ExitStack
        with ExitStack() as es:
            def sbt(name, shape, dt):
                return es.enter_context(nc.sbuf_tensor(self.un(name), list(shape), dt))
            cT_s = sbt("cT_s", [128, NCH, 2], F32)
            sT = sbt("sT", [128, NCH, 2], BF16)
            modrow = sbt("modrow", [2, 6 * D], F32)
            brow = sbt("brow", [2, 6 * D], F32)
            wsl = [sbt("wmod_sl%d" % i, [128, NCH, 512], BF16) for i in range(2)]
            b_c, b_s, b_mr, b_br = kb.buf(), kb.buf(), kb.buf(), kb.buf()
            b_w = [kb.buf(), kb.buf()]
            kb.dma("sp", cT_s[:], cT[:, :, :], [], [b_c])
            kb.op("act", [b_c], [b_s], lambda E: E.activation(out=sT[:], in_=cT_s[:], func=AF.Silu))
            for l in range(DEPTH):
                for r in range(2):
                    kb.dma("sp", brow[r:r + 1, :], b_mod[l:l + 1, :], [], [b_br])
                wv = w_mod[l].rearrange("(k p) n -> p k n", p=128)
                for j in range(24):
                    s = j % 2
                    kb.dma("pool", wsl[s][:], wv[:, :, j * 512:(j + 1) * 512], [], [b_w[s]])
                    ps = self.psum[s]
                    for k in range(NCH):
                        self.mm(ps[0:2, :], sT[:, k, :], wsl[s][:, k, :], k == 0, k == NCH - 1,
                                [b_s, b_w[s]], [self.b_ps[s]])
                    kb.op("dve", [self.b_ps[s], b_br], [b_mr], lambda E: E.tensor_tensor(
                        out=modrow[:, j * 512:(j + 1) * 512], in0=ps[0:2, :], in1=brow[:, j * 512:(j + 1) * 512],
                        op=ALU.add))
                pst = self.psum[2]
                for cc in range(96):
                    self.mm(pst[:, cc * 2:cc * 2 + 2], modrow[0:2, cc * 128:(cc + 1) * 128], self.ident_f[0:2, 0:2],
                            True, True, [b_mr, self.b_const], [self.b_ps[2]])
                kb.op("dve", [self.b_ps[2]], [self.b_modT], lambda E: E.tensor_copy(
                    out=self.modT[:, l, :, :], in_=pst[:, 0:192].rearrange("p (a b) -> p a b", b=2)))
            kb.barrier()

    def phase_norm_test(self):
        pass


def expand_bias(rpb):
    L, H = rpb.shape[0], rpb.shape[1]
    flat = np.concatenate([rpb.reshape(L, H, -1), np.full((L, H, 1), NEG, np.float32)], axis=2)
    idx = np.full((5, 128, 576), 15 * 31, np.int64)
    pats = [(0, 0), (2, 0), (None, None), (28, 24), (30, 24)]
    for pi, (rq0, k0) in enumerate(pats):
        if rq0 is None:
            rq0, k0 = 8, 4
        for ql in range(128):
            r = rq0 + ql // 64
            cq = ql % 64
            r0 = min(max(r - 4, 0), 24)
            ws = min(max(cq - 8, 0), 48)
            for krl in range(9):
                kr = k0 + krl
                if not (r0 <= kr < r0 + 8) or kr > 31:
                    continue
                for ck in range(ws, ws + 16):
                    dc = min(max(ck - cq + 15, 0), 30)
                    idx[pi, ql, krl * 64 + ck] = (kr - r + 7) * 31 + dc
    out = flat[:, :, idx]
    return np.ascontiguousarray(out.astype(np.float32))


def host_prep(inputs):
    x = np.asarray(inputs["x"], np.float32)
    ctx = np.asarray(inputs["ctx"], np.float32)
    c = np.asarray(inputs["c"], np.float32)
    c_ctx = np.asarray(inputs["c_ctx"], np.float32)
    shared = {}
    shared["w_mod"] = np.ascontiguousarray(inputs["w_mod"], np.float32)
    shared["b_mod"] = np.ascontiguousarray(inputs["b_mod"], np.float32)
    ng = np.asarray(inputs["norm_gain"], np.float32)
    shared["ngT"] = np.ascontiguousarray(ng.reshape(DEPTH, 2, NCH, 128).transpose(3, 0, 1, 2))
    shared["wr"] = np.ascontiguousarray(np.concatenate([inputs["moe_w_rg"], inputs["moe_w_re"]], axis=2), np.float32)
    shared["br"] = np.ascontiguousarray(np.concatenate([inputs["moe_b_rg"], inputs["moe_b_re"]], axis=1), np.float32)
    for k in ("moe_w_gate", "moe_w_up", "moe_w_down"):
        shared[k] = np.ascontiguousarray(inputs[k], np.float32)
    import ml_dtypes
    sel = np.zeros((32, 32, 128), ml_dtypes.bfloat16)
    for e in range(32):
        sel[e, e, :] = 1.0
    shared["sel32"] = sel
    shared["odd_w_in"] = np.ascontiguousarray(inputs["odd_w_in"], np.float32)
    shared["odd_w_out"] = np.ascontiguousarray(inputs["odd_w_out"], np.float32)
    shared["odd_wsT"] = np.ascontiguousarray(np.asarray(inputs["odd_w_s"], np.float32).transpose(0, 3, 1, 2))
    shared["odd_bs"] = np.ascontiguousarray(np.asarray(inputs["odd_b_s"], np.float32).reshape(2, 1024))
    shared["odd_vg"] = np.ascontiguousarray(np.asarray(inputs["odd_v_gain"], np.float32).reshape(2, 32, 128).transpose(0, 2, 1))
    shared["even_w_in"] = np.ascontiguousarray(inputs["even_w_in"], np.float32)
    shared["even_w_out"] = np.ascontiguousarray(inputs["even_w_out"], np.float32)
    ec = np.asarray(inputs["even_conv"], np.float32)
    shared["convT"] = np.ascontiguousarray(ec.reshape(2, 3, 16, 128).transpose(0, 3, 2, 1))
    shared["qkgT"] = np.ascontiguousarray(np.asarray(inputs["even_qk_gain"], np.float32).transpose(0, 2, 1))
    shared["gate_bT"] = np.ascontiguousarray(np.asarray(inputs["even_gate_b"], np.float32).reshape(2, 4, 8).transpose(0, 2, 1))
    shared["head_gain"] = np.ascontiguousarray(inputs["even_head_gain"], np.float32)
    shared["biasx"] = expand_bias(np.asarray(inputs["even_rpb"], np.float32))
    t = np.arange(128)
    same = (t[:, None] // 64) == (t[None, :] // 64)
    mF = np.where(same & (t[:, None] <= t[None, :]), 0.0, NEG).astype(np.float32)
    mR = np.where(same & (t[:, None] >= t[None, :]), 0.0, NEG).astype(np.float32)
    shared["maskT"] = np.stack([mF, mR])
    s8 = np.zeros((8, 8, 128), np.float32)
    for e in range(8):
        s8[e, e, :] = 1.0
    shared["sel8"] = s8
    shared["Jm"] = np.ascontiguousarray(np.eye(128, dtype=np.float32)[::-1])
    nonce = np.full((1, 4), int(np.random.randint(1, 2 ** 30)), np.int32)
    shared["nonce"] = nonce
    per = []
    for core in range(8):
        b, sl = core // 2, core % 2
        tok = np.concatenate([x[b, sl * 1024:(sl + 1) * 1024], ctx[b, sl * 128:(sl + 1) * 128]], axis=0)
        xT = np.ascontiguousarray(tok.reshape(NLOC, NCH, 128).transpose(2, 1, 0))
        cc = np.stack([c[b], c_ctx], axis=1)
        cT = np.ascontiguousarray(cc.reshape(NCH, 128, 2).transpose(1, 0, 2))
        d = dict(shared)
        d["xT"] = xT
        d["cT"] = cT
        per.append(d)
    return per


def run(inputs, upto="all", ncores=8, trace=False):
    p = Prog(upto)
    p.build()
    per = host_prep(inputs)
    in_maps = [{k: per[i][k] for k in p.T} for i in range(ncores)]
    res = run_bass_kernel_spmd(p.nc, in_maps, core_ids=list(range(ncores)), trace=trace)
    return res


def kernel(**inputs):
    res = run(inputs, ncores=8)
    outp = np.empty((4, NL, D), np.float32)
    for core in range(8):
        b, sl = core // 2, core % 2
        o = np.asarray(res.results[core]["out"])
        outp[b, sl * 1024:(sl + 1) * 1024] = o.transpose(2, 1, 0).reshape(1024, D)
    return outp
```

```python
import numpy as np
import concourse.bass as bass
import concourse.mybir as mybir
from concourse.bass_utils import run_bass_kernel_spmd

F32 = mybir.dt.float32
BF16 = mybir.dt.bfloat16
AF = mybir.ActivationFunctionType
ALU = mybir.AluOpType
AX = mybir.AxisListType

D = 2048
NCH = 16
NL = 2048
NCX = 256
NT = NL + NCX
DEPTH = 4
EPS = 1e-6
NEG = -1e30
NDS = 40
NLOC = 1152
I32 = mybir.dt.int32


class Tok:
    __slots__ = ("sem", "key", "val", "eng")

    def __init__(self, sem, key, val, eng):
        self.sem, self.key, self.val, self.eng = sem, key, val, eng


class Buf:
    __slots__ = ("name", "w", "r")

    def __init__(self, name):
        self.name, self.w, self.r = name, None, {}


class KB:
    def __init__(self):
        nc = bass.Bass("TRN2", target_bir_lowering=False, num_devices=8)
        self.nc = nc
        self.eng = {"pe": nc.tensor, "act": nc.scalar, "dve": nc.vector, "pool": nc.gpsimd, "sp": nc.sync}
        self.sem = {k: nc.alloc_semaphore("s_" + k) for k in ("pe", "act", "dve", "pool")}
        self.cnt = {k: 0 for k in self.eng}
        self.seen = {k: {} for k in self.eng}
        self.dsem = [nc.alloc_semaphore("d%d" % i) for i in range(NDS)]
        self.dcnt = [0] * NDS
        self.dnext = 0
        self.nbuf = 0

    def buf(self, name=None):
        self.nbuf += 1
        return Buf(name or "b%d" % self.nbuf)

    def _waits(self, e, reads, writes, is_dma):
        waits = {}

        def need(tok, raw):
            if tok is None:
                return
            if (not is_dma) and tok.eng == e:
                if e == "pe" or not raw:
                    return
                if self.cnt[e] - tok.val >= 6:
                    return
            cur = waits.get(tok.key)
            if cur is None or cur.val < tok.val:
                waits[tok.key] = tok

        for b in reads:
            need(b.w, True)
        for b in writes:
            need(b.w, False)
            for t in b.r.values():
                need(t, False)
        E = self.eng[e]
        seen = self.seen[e]
        for key, tok in waits.items():
            if seen.get(key, 0) >= tok.val:
                continue
            E.wait_ge(tok.sem, tok.val)
            seen[key] = tok.val

    def _mark(self, tok, reads, writes):
        for b in writes:
            b.w = tok
            b.r = {}
        for b in reads:
            if b in writes:
                continue
            cur = b.r.get(tok.key)
            if cur is None or cur.val < tok.val:
                b.r[tok.key] = tok

    def op(self, e, reads, writes, fn):
        self._waits(e, reads, writes, False)
        ins = fn(self.eng[e])
        self.cnt[e] += 1
        ins.then_inc(self.sem[e], 1)
        tok = Tok(self.sem[e], e, self.cnt[e], e)
        self._mark(tok, reads, writes)
        return tok

    def dma(self, q, out, in_, reads, writes, **kw):
        j = self.dnext
        self.dnext = (self.dnext + 1) % NDS
        E = self.eng[q]
        key = "d%d" % j
        if self.dcnt[j] > 0 and self.seen[q].get(key, 0) < self.dcnt[j]:
            E.wait_ge(self.dsem[j], self.dcnt[j])
            self.seen[q][key] = self.dcnt[j]
        self._waits(q, reads, writes, True)
        if callable(out) or callable(in_):
            assert q == "sp"
            for sl in (0, 1):
                self.prog.slot = sl
                o = out() if callable(out) else out
                i = in_() if callable(in_) else in_
                guard = E.If_eq(self.slot_reg, 0) if sl == 0 else E.Else()
                with guard:
                    E.dma_start(out=o, in_=i, **kw).then_inc(self.dsem[j], 16)
            self.prog.slot = None
        else:
            ins = E.dma_start(out=out, in_=in_, **kw)
            ins.then_inc(self.dsem[j], 16)
        self.dcnt[j] += 16
        tok = Tok(self.dsem[j], key, self.dcnt[j], None)
        self._mark(tok, reads, writes)
        return tok

    def barrier(self):
        for e in self.eng:
            E = self.eng[e]
            for o in ("pe", "act", "dve", "pool"):
                if o != e and self.cnt[o] > self.seen[e].get(o, 0):
                    E.wait_ge(self.sem[o], self.cnt[o])
                    self.seen[e][o] = self.cnt[o]
            for j in range(NDS):
                key = "d%d" % j
                if self.dcnt[j] > self.seen[e].get(key, 0):
                    E.wait_ge(self.dsem[j], self.dcnt[j])
                    self.seen[e][key] = self.dcnt[j]

    def pair_barrier(self, flags, nonce, slot, other):
        nc = self.nc
        sp = self.eng["sp"]
        if not hasattr(self, "bar_sem"):
            self.bar_sem = nc.alloc_semaphore("pairbar")
            self.bar_cnt = 0
            self.bar_regs = (sp.alloc_register("nreg"), sp.alloc_register("treg"), sp.alloc_register("creg"))
            sp.load(self.bar_regs[0], nonce[0:1, 0:1])
        nreg, treg, creg = self.bar_regs
        for o in ("pe", "act", "dve", "pool"):
            if self.cnt[o] > self.seen["sp"].get(o, 0):
                sp.wait_ge(self.sem[o], self.cnt[o])
                self.seen["sp"][o] = self.cnt[o]
        for j in range(NDS):
            key = "d%d" % j
            if self.dcnt[j] > self.seen["sp"].get(key, 0):
                sp.wait_ge(self.dsem[j], self.dcnt[j])
                self.seen["sp"][key] = self.dcnt[j]
        ev = self.bar_cnt
        for sl in (0, 1):
            guard = sp.If_eq(self.slot_reg, 0) if sl == 0 else sp.Else()
            with guard:
                sp.store(flags[ev, sl:sl + 1, 0:1], nreg)
                sp.reg_mov(creg, 1)
                with sp.While(creg):
                    sp.load(treg, flags[ev, 1 - sl:2 - sl, 0:1])
                    sp.reg_sub(creg, treg, nreg)
        self.bar_cnt += 1
        sp.sem_inc(self.bar_sem, 1)
        for e in ("pe", "act", "dve", "pool"):
            self.eng[e].wait_ge(self.bar_sem, self.bar_cnt)

    def final_wait(self, toks):
        E = self.eng["sp"]
        for t in toks:
            E.wait_ge(t.sem, t.val)


class Prog:
    def __init__(self, upto="all"):
        self.kb = KB()
        self.nc = self.kb.nc
        self.upto = upto
        self.T = {}

    def un(self, name):
        self._uid = getattr(self, "_uid", 0) + 1
        return "%s_%d" % (name, self._uid)

    def din(self, name, shape, dt=F32):
        t = self.nc.dram_tensor(name, list(shape), dt, kind="ExternalInput").ap()
        self.T[name] = t
        return t

    def dscratch(self, name, shape, dt):
        return self.nc.dram_tensor(name, list(shape), dt, kind="Internal").ap()

    def sb(self, name, shape, dt):
        return self.nc.alloc_sbuf_tensor(name, list(shape), dt)

    def dshared(self, name, shape, dt):
        return self.nc.dram_tensor(name, list(shape), dt, kind="Internal", addr_space="Shared").ap()

    def tk(self, ap, gi):
        nd = len(ap.shape)
        pre = "p c" if nd == 3 else "p"
        lead = (slice(None),) * (nd - 1)
        if gi == 2:
            v = ap[lead + (slice(NL, NT),)].rearrange("%s (s t) -> %s s t" % (pre, pre), s=2)
            v = v[lead + (bass.ts(self.slot, 1),)]
            return v.rearrange("%s s t -> %s (s t)" % (pre, pre))
        v = ap[lead + (slice(0, NL),)].rearrange("%s (s t) -> %s s t" % (pre, pre), s=2)
        v = v[lead + (bass.ts(self.slot, 1),)]
        if gi == "lat":
            return v.rearrange("%s s t -> %s (s t)" % (pre, pre))
        v = v[lead + (slice(None), slice(gi * 512, (gi + 1) * 512))]
        return v.rearrange("%s s t -> %s (s t)" % (pre, pre))

    def hd3(self, ap, w, hh):
        v = ap.rearrange("(w s e) p t -> w s e p t", w=2, s=2, e=4)[w, bass.ts(self.slot, 1), hh]
        return v.rearrange("s p t -> p (s t)")

    def vcols(self, a, hh):
        v = self.V_d.rearrange("(T p) (a s e j) -> p T a s e j", p=128, a=3, s=2, e=4)[:, :, a, bass.ts(self.slot, 1), hh, :]
        return v.rearrange("p T s j -> p T (s j)")

    def pbar(self):
        self.kb.barrier()
        self.kb.pair_barrier(self.flags, self.nonce, self.slot_rt, self.other_rt)

    def mm(self, out, lhsT, rhs, start, stop, reads, writes):
        return self.kb.op("pe", reads, writes, lambda E: E.matmul(out, lhsT, rhs, start=start, stop=stop))

    def build(self):
        nc, kb = self.nc, self.kb
        xT = self.din("xT", [128, NCH, NLOC])
        self.nonce = self.din("nonce", [1, 4], I32)
        self.flags = self.dshared("flags", [8, 2, 4], I32)
        pid = nc.sync.partition_id()
        self.slot_rt = pid % 2
        self.other_rt = None
        kb.slot_reg = nc.sync.to_reg(self.slot_rt)
        kb.prog = self
        self.slot = None
        cT = self.din("cT", [128, NCH, 2])
        w_mod = self.din("w_mod", [DEPTH, D, 6 * D])
        b_mod = self.din("b_mod", [DEPTH, 6 * D])
        ngT = self.din("ngT", [128, DEPTH, 2, NCH])
        out = nc.dram_tensor("out", [128, NCH, 1024], F32, kind="ExternalOutput").ap()
        self.out = out
        hT_d = self.dshared("hT_d", [128, NCH, NT], F32)
        self.hT_d = hT_d
        self.b_hT = [kb.buf("hT_d%d" % i) for i in range(3)]
        self.LG = [(0, 512, 0, 0), (512, 512, 0, 1), (1024, 128, 1, 2)]
        self.b_out = kb.buf("out")

        self.ones_f = self.sb("ones_f", [128, 128], F32)
        self.ident_f = self.sb("ident_f", [128, 128], F32)
        self.modT = self.sb("modT", [128, DEPTH, 96, 2], F32)
        self.ngs = self.sb("ngs", [128, DEPTH, 2, NCH], F32)
        self.ident_b = self.sb("ident_b", [128, 128], BF16)
        self.b_const = kb.buf("const")
        self.b_modT = kb.buf("modT")
        kb.op("dve", [], [self.b_const], lambda E: E.memset(self.ones_f[:], 1.0))
        kb.op("dve", [], [self.b_const], lambda E: E.memset(self.ident_f[:], 1.0))
        kb.op("pool", [self.b_const], [self.b_const], lambda E: E.affine_select(
            out=self.ident_f[:], in_=self.ident_f[:], pattern=[[-1, 128]], compare_op=ALU.is_equal,
            fill=0.0, base=0, channel_multiplier=1))
        kb.op("dve", [self.b_const], [self.b_const], lambda E: E.tensor_copy(out=self.ident_b[:], in_=self.ident_f[:]))
        kb.dma("sp", self.ngs[:], ngT[:, :, :, :], [], [self.b_const])
        self.psum = [nc.alloc_psum_tensor("ps%d" % i, [128, 512], F32) for i in range(8)]
        self.b_ps = [kb.buf("ps%d" % i) for i in range(8)]

        self.phase_mod(cT, w_mod, b_mod)
        if self.upto == "mod":
            kb.barrier()
            t = kb.dma("sp", out[:, 0, 0:768], self.modT[:].rearrange("p l a c -> p (l a c)"),
                       [self.b_modT], [self.b_out])
            kb.final_wait([t])
            return
        for (lo, n, col, g) in self.LG:
            kb.dma("sp", lambda: self.tk(hT_d, g), xT[:, :, lo:lo + n], [], [self.b_hT[g]])
        kb.barrier()
        self.declare_weights()
        if self.upto == "moe_test":
            self.phase_moe(0, True)
        elif self.upto == "odd_test":
            self.phase_odd(1, True)
        elif self.upto == "even_test":
            self.phase_even(0, True)
        else:
            for l in range(DEPTH):
                need_ctx = l < 2
                if l % 2 == 0:
                    self.phase_even(l, need_ctx)
                else:
                    self.phase_odd(l, need_ctx)
                self.phase_moe(l, need_ctx)
        kb.barrier()
        toks = [kb.dma("sp", out[:, :, :], lambda: self.tk(hT_d, "lat"), [self.b_hT[0], self.b_hT[1]], [self.b_out])]
        kb.final_wait(toks)

    def declare_weights(self):
        self.wr = self.din("wr", [DEPTH, D, 36])
        self.br = self.din("br", [DEPTH, 36])
        self.w_gate = self.din("moe_w_gate", [DEPTH, 32, D, 512])
        self.w_up = self.din("moe_w_up", [DEPTH, 32, D, 512])
        self.w_down = self.din("moe_w_down", [DEPTH, 32, 512, D])
        self.sel32 = self.din("sel32", [32, 32, 128], BF16)
        self.even_w_in = self.din("even_w_in", [2, D, 7200])
        self.even_w_out = self.din("even_w_out", [2, D, D])
        self.convT = self.din("convT", [2, 128, 16, 3])
        self.qkgT = self.din("qkgT", [2, 128, 2])
        self.gate_bT = self.din("gate_bT", [2, 8, 4])
        self.head_gain = self.din("head_gain", [2, 1024])
        self.biasx = self.din("biasx", [2, 8, 5, 128, 576])
        self.maskT = self.din("maskT", [2, 128, 128])
        self.sel8 = self.din("sel8", [8, 8, 128])
        self.Jm = self.din("Jm", [128, 128])
        self.QK_d = self.dshared("QK_d", [16, 128, NT], BF16)
        self.QR_d = self.dshared("QR_d", [16, 128, NT], F32)
        self.V_d = self.dshared("V_d", [NT, 3072], BF16)
        self.G_d = self.dshared("G_d", [NT, 32], F32)
        self.YC_d = self.dshared("YC_d", [16, 128, NT], BF16)
        self.odd_w_in = self.din("odd_w_in", [2, D, 8192])
        self.odd_w_out = self.din("odd_w_out", [2, 4096, D])
        self.odd_wsT = self.din("odd_wsT", [2, 128, 8, 128])
        self.odd_bs = self.din("odd_bs", [2, 1024])
        self.odd_vg = self.din("odd_vg", [2, 128, 32])

    def norm_tile(self, l, which, col, h_ap, n, out_ap, b_h, b_out, tmp, b_tmp, psb, f32_out=None, b_f32=None, chunk_cb=None):
        kb = self.kb
        ps = self.psum[psb]
        bps = self.b_ps[psb]
        sq, rstd, t1, AB = tmp["sq"], tmp["rstd"], tmp["t1"], tmp["AB"]
        sh = which * 3
        kb.op("dve", [self.b_modT, self.b_const], [b_tmp["AB"]], lambda E: E.scalar_tensor_tensor(
            out=AB[:, 0, :], in0=self.modT[:, l, (sh + 1) * 16:(sh + 2) * 16, col], scalar=1.0,
            in1=self.ngs[:, l, which, :], op0=ALU.add, op1=ALU.mult))
        for c in range(NCH):
            s = c % 2
            if c % 2 == 0:
                kb.op("act", [b_h], [b_tmp["sq%d" % s]], lambda E: E.activation(
                    out=sq[s][:, :n], in_=h_ap[:, c, :], func=AF.Square))
            else:
                kb.op("pool", [b_h], [b_tmp["sq%d" % s]], lambda E: E.tensor_tensor(
                    out=sq[s][:, :n], in0=h_ap[:, c, :], in1=h_ap[:, c, :], op=ALU.mult))
            self.mm(ps[:, :n], self.ones_f[:], sq[s][:, :n], c == 0, c == NCH - 1,
                    [self.b_const, b_tmp["sq%d" % s]], [bps])
        kb.op("act", [bps], [b_tmp["rstd"]], lambda E: E.activation(
            out=rstd[:, :n], in_=ps[:, :n], func=AF.Sqrt, bias=EPS, scale=1.0 / D))
        kb.op("dve", [b_tmp["rstd"]], [b_tmp["rstd"]], lambda E: E.reciprocal(out=rstd[:, :n], in_=rstd[:, :n]))
        for c in range(NCH):
            s = c % 2
            kb.op("dve", [b_h, b_tmp["rstd"], b_tmp["AB"]], [b_tmp["t1%d" % s]], lambda E: E.scalar_tensor_tensor(
                out=t1[s][:, :n], in0=h_ap[:, c, :], scalar=AB[:, 0, c:c + 1], in1=rstd[:, :n],
                op0=ALU.mult, op1=ALU.mult))
            if f32_out is None:
                kb.op("act", [b_tmp["t1%d" % s], self.b_modT], [b_out], lambda E: E.activation(
                    out=out_ap[:, c, :], in_=t1[s][:, :n], func=AF.Identity,
                    bias=self.modT[:, l, sh * 16 + c, col:col + 1], scale=1.0))
            else:
                kb.op("act", [b_tmp["t1%d" % s], self.b_modT], [b_f32[s]], lambda E: E.activation(
                    out=f32_out[s][:, :n], in_=t1[s][:, :n], func=AF.Identity,
                    bias=self.modT[:, l, sh * 16 + c, col:col + 1], scale=1.0))
                kb.op("pool", [b_f32[s]], [b_out], lambda E: E.tensor_copy(out=out_ap[:, c, :], in_=f32_out[s][:, :n]))
                chunk_cb(c, f32_out[s], b_f32[s])

    def norm_tmp(self, sbt):
        kb = self.kb
        tmp = {"sq": [sbt("n_sq0", [128, 512], F32), sbt("n_sq1", [128, 512], F32)],
               "rstd": sbt("n_rstd", [128, 512], F32),
               "t1": [sbt("n_t10", [128, 512], F32), sbt("n_t11", [128, 512], F32)],
               "AB": sbt("n_AB", [128, 2, NCH], F32)}
        b = {k: kb.buf() for k in ("sq0", "sq1", "rstd", "t10", "t11", "AB")}
        return tmp, b

    def phase_even(self, l, need_ctx):
        nc, kb = self.nc, self.kb
        from contextlib import ExitStack
        i = l // 2
        w_in = self.even_w_in[i].rearrange("(k p) f -> p k f", p=128)
        LG = self.LG
        b_qk, b_qr, b_vd, b_gd, b_yc = kb.buf(), kb.buf(), kb.buf(), kb.buf(), kb.buf()
        QK_d, QR_d, V_d, G_d, YC_d = self.QK_d, self.QR_d, self.V_d, self.G_d, self.YC_d
        with ExitStack() as es:
            def sbt(name, shape, dt):
                return es.enter_context(nc.sbuf_tensor(self.un(name), list(shape), dt))
            xm = sbt("e_xm", [128, NCH, NLOC], BF16)
            hg = sbt("e_h", [128, NCH, 512], F32)
            tmp, b_tmp = self.norm_tmp(sbt)
            b_xm = [kb.buf() for _ in range(3)]
            b_h = kb.buf()
            for (lo, n, col, g) in LG:
                kb.dma("sp", hg[:, :, :n], lambda: self.tk(self.hT_d, g), [self.b_hT[g]], [b_h])
                self.norm_tile(l, 0, col, hg[:, :, :n], n, xm[:, :, lo:lo + n], b_h, b_xm[g], tmp, b_tmp, 0)
            wf = [sbt("e_wf%d" % k, [128, NCH, 128], BF16) for k in range(2)]
            wt = [sbt("e_wt%d" % k, [128, NCH, 512], BF16) for k in range(2)]
            raw = [sbt("e_raw%d" % k, [128, NLOC], F32) for k in range(2)]
            ob = [sbt("e_ob%d" % k, [128, NLOC], BF16) for k in range(2)]
            stg = sbt("e_stg", [128, 9, 512], BF16)
            gst = sbt("e_gst", [128, 9, 32], F32)
            qg = sbt("e_qg", [128, 2], F32)
            b_wf, b_wt = [kb.buf(), kb.buf()], [kb.buf(), kb.buf()]
            b_raw, b_ob = [kb.buf(), kb.buf()], [kb.buf(), kb.buf()]
            b_stg, b_gst, b_c = kb.buf(), kb.buf(), kb.buf()
            kb.dma("sp", qg[:], self.qkgT[i], [], [b_c])
            kb.op("dve", [b_c], [b_c], lambda E: E.tensor_scalar(out=qg[:, 0:1], in0=qg[:, 0:1], scalar1=128.0 ** -0.5,
                                                                 scalar2=None, op0=ALU.mult))
            sq, rstd = tmp["sq"], tmp["rstd"]
            for cc in range(32):
                s_ = cc % 2
                c0 = cc * 128 if cc < 8 else (1024 + (cc - 8) * 128 if cc < 16 else (3072 + (cc - 16) * 128 if cc < 24 else 4096 + (cc - 24) * 128))
                kb.dma("pool", wf[s_][:], w_in[:, :, c0:c0 + 128], [], [b_wf[s_]])
                for (lo, n, col, g) in LG:
                    pb = 1 + g % 2
                    ps = self.psum[pb]
                    for k in range(NCH):
                        self.mm(ps[:, :n], wf[s_][:, k, :], xm[:, k, lo:lo + n], k == 0, k == NCH - 1, [b_wf[s_], b_xm[g]], [self.b_ps[pb]])
                    kb.op("act", [self.b_ps[pb]], [b_raw[s_]], lambda E: E.activation(out=raw[s_][:, lo:lo + n], in_=ps[:, :n], func=AF.Copy))
                if cc < 16:
                    which = 0 if cc < 8 else 1
                    for (lo, n, col, g) in LG:
                        kb.op("act", [b_raw[s_]], [b_tmp["sq0"]], lambda E: E.activation(out=sq[0][:, :n], in_=raw[s_][:, lo:lo + n], func=AF.Square))
                        self.mm(self.psum[3][:, :n], self.ones_f[:], sq[0][:, :n], True, True, [self.b_const, b_tmp["sq0"]], [self.b_ps[3]])
                        kb.op("act", [self.b_ps[3]], [b_tmp["rstd"]], lambda E: E.activation(
                            out=rstd[:, :n], in_=self.psum[3][:, :n], func=AF.Sqrt, bias=EPS, scale=1.0 / 128))
                        kb.op("dve", [b_tmp["rstd"]], [b_tmp["rstd"]], lambda E: E.reciprocal(out=rstd[:, :n], in_=rstd[:, :n]))
                        kb.op("dve", [b_raw[s_], b_tmp["rstd"], b_c], [b_ob[s_]], lambda E: E.scalar_tensor_tensor(
                            out=ob[s_][:, lo:lo + n], in0=raw[s_][:, lo:lo + n], scalar=qg[:, which:which + 1], in1=rstd[:, :n],
                            op0=ALU.mult, op1=ALU.mult))
                    kb.dma("sp", lambda: self.tk(QK_d[cc], "lat"), ob[s_][:, 0:1024], [b_ob[s_]], [b_qk])
                    kb.dma("sp", lambda: self.tk(QK_d[cc], 2), ob[s_][:, 1024:NLOC], [b_ob[s_]], [b_qk])
                else:
                    kb.dma("sp", lambda: self.tk(QR_d[cc - 16], "lat"), raw[s_][:, 0:1024], [b_raw[s_]], [b_qr])
                    kb.dma("sp", lambda: self.tk(QR_d[cc - 16], 2), raw[s_][:, 1024:NLOC], [b_raw[s_]], [b_qr])
            Vv = V_d.rearrange("(T p) c -> p T c", p=128)
            Gv = G_d.rearrange("(T p) c -> p T c", p=128)
            for ct in range(7):
                s_ = ct % 2
                c0 = [2048, 2560, 5120, 5632, 6144, 6656, 7168][ct]
                m = 512 if ct < 6 else 32
                kb.dma("pool", wt[s_][:, :, :m], w_in[:, :, c0:c0 + m], [], [b_wt[s_]])
                for T in range(9):
                    pb = 1 + T % 2
                    ps = self.psum[pb]
                    g = min(T // 4, 2)
                    for k in range(NCH):
                        self.mm(ps[:, :m], xm[:, k, T * 128:(T + 1) * 128], wt[s_][:, k, :m], k == 0, k == NCH - 1,
                                [b_xm[g], b_wt[s_]], [self.b_ps[pb]])
                    if ct < 4:
                        kb.op("act", [self.b_ps[pb]], [b_stg], lambda E: E.activation(out=stg[:, T, :], in_=ps[:, :512], func=AF.Copy))
                    elif ct < 6:
                        kb.op("act", [self.b_ps[pb]], [b_stg], lambda E: E.activation(out=stg[:, T, :], in_=ps[:, :512], func=AF.Sigmoid))
                    else:
                        kb.op("act", [self.b_ps[pb]], [b_gst], lambda E: E.activation(out=gst[:, T, :], in_=ps[:, :32], func=AF.Copy))
                if ct < 6:
                    kb.dma("sp", lambda: Vv[:, 0:16, ct * 512:(ct + 1) * 512].rearrange("p (s t) c -> p s t c", s=2)[:, bass.ts(self.slot, 1)],
                           stg[:, 0:8, :].rearrange("p (s t) c -> p s t c", s=1), [b_stg], [b_vd])
                    kb.dma("sp", lambda: Vv[:, 16:18, ct * 512:(ct + 1) * 512][:, bass.ts(self.slot, 1)], stg[:, 8:9, :], [b_stg], [b_vd])
                else:
                    kb.dma("sp", lambda: Gv[:, 0:16, :].rearrange("p (s t) c -> p s t c", s=2)[:, bass.ts(self.slot, 1)],
                           gst[:, 0:8, :].rearrange("p (s t) c -> p s t c", s=1), [b_gst], [b_gd])
                    kb.dma("sp", lambda: Gv[:, 16:18, :][:, bass.ts(self.slot, 1)], gst[:, 8:9, :], [b_gst], [b_gd])
        self.pbar()
        self.phase_na(l, i, need_ctx, b_qk, b_vd, b_yc)
        self.phase_mlstm(l, i, b_qr, b_vd, b_gd, b_yc)
        self.pbar()
        with ExitStack() as es:
            def sbt(name, shape, dt):
                return es.enter_context(nc.sbuf_tensor(self.un(name), list(shape), dt))
            yc = sbt("o_yc", [128, 16, NLOC], BF16)
            wo = [sbt("o_w%d" % k, [128, 16, 128], BF16) for k in range(2)]
            ht = [sbt("o_h%d" % k, [128, 512], F32) for k in range(2)]
            b_ycs, b_wo, b_ht = kb.buf(), [kb.buf(), kb.buf()], [kb.buf(), kb.buf()]
            for c in range(16):
                kb.dma("sp", yc[:, c, 0:1024], lambda: self.tk(YC_d[c], "lat"), [b_yc], [b_ycs])
                kb.dma("sp", yc[:, c, 1024:NLOC], lambda: self.tk(YC_d[c], 2), [b_yc], [b_ycs])
            w_out = self.even_w_out[i].rearrange("(k p) d -> p k d", p=128)
            it = 0
            for dc in range(NCH):
                s_ = dc % 2
                kb.dma("pool", wo[s_][:], w_out[:, :, dc * 128:(dc + 1) * 128], [], [b_wo[s_]])
                for (lo, n, col, g) in LG:
                    if g == 2 and not need_ctx:
                        continue
                    hs = it % 2
                    it += 1
                    kb.dma("sp", ht[hs][:, :n], lambda: self.tk(self.hT_d[:, dc, :], g), [self.b_hT[g]], [b_ht[hs]])
                    ps = self.psum[1 + hs]
                    for k in range(16):
                        self.mm(ps[:, :n], wo[s_][:, k, :], yc[:, k, lo:lo + n], k == 0, k == 15, [b_wo[s_], b_ycs], [self.b_ps[1 + hs]])
                    kb.op("dve", [self.b_ps[1 + hs], b_ht[hs], self.b_modT], [b_ht[hs]], lambda E: E.scalar_tensor_tensor(
                        out=ht[hs][:, :n], in0=ps[:, :n], scalar=self.modT[:, l, 2 * 16 + dc, col:col + 1], in1=ht[hs][:, :n],
                        op0=ALU.mult, op1=ALU.add))
                    kb.dma("sp", lambda: self.tk(self.hT_d[:, dc, :], g), ht[hs][:, :n], [b_ht[hs]], [self.b_hT[g]])
            kb.barrier()

    def phase_na(self, l, i, need_ctx, b_qk, b_vd, b_yc):
        nc, kb = self.nc, self.kb
        from contextlib import ExitStack
        with ExitStack() as es:
            def sbt(name, shape, dt):
                return es.enter_context(nc.sbuf_tensor(self.un(name), list(shape), dt))
            qT = sbt("a_q", [128, NT], BF16)
            kT = sbt("a_k", [128, NT], BF16)
            va = sbt("a_v", [128, 18, 128], BF16)
            bia = sbt("a_b", [128, 5, 576], F32)
            Sb = sbt("a_S", [128, 832], F32)
            Pn = sbt("a_P", [128, 832], BF16)
            PT = sbt("a_PT", [128, 7, 128], BF16)
            oTs = sbt("a_o", [128, NT], BF16)
            st = sbt("a_st", [128, 4], F32)
            b_q, b_k, b_v, b_b, b_S, b_P, b_PT, b_o, b_st = [kb.buf() for _ in range(9)]
            psTb = self.psum[3][:].bitcast(BF16)
            for hh in range(4):
                kb.dma("sp", qT[:], lambda: self.hd3(self.QK_d, 0, hh), [b_qk], [b_q])
                kb.dma("sp", kT[:], lambda: self.hd3(self.QK_d, 1, hh), [b_qk], [b_k])
                kb.dma("sp", va[:], lambda: self.vcols(0, hh), [b_vd], [b_v])
                kb.dma("sp", bia[:], lambda: self.biasx[i].rearrange("(s e) q p k -> s e q p k", s=2)[bass.ts(self.slot, 1), hh]
                       .rearrange("s q p k -> p (s q) k"), [], [b_b])
                if not need_ctx:
                    kb.op("dve", [], [b_o], lambda E: E.memset(oTs[:, NL:NT], 0.0))
                for T in range(18 if need_ctx else 16):
                    q = qT[:, T * 128:(T + 1) * 128]
                    psA, psB = self.psum[1], self.psum[2]
                    if T < 16:
                        ks = min(max(T - 2, 0), 12)
                        pat = {0: 0, 1: 1, 14: 3, 15: 4}.get(T, 2)
                        self.mm(psA[:, 0:512], q, kT[:, ks * 128:ks * 128 + 512], True, True, [b_q, b_k], [self.b_ps[1]])
                        self.mm(psB[:, 0:64], q, kT[:, ks * 128 + 512:ks * 128 + 576], True, True, [b_q, b_k], [self.b_ps[2]])
                        self.mm(psB[:, 64:320], q, kT[:, NL:NT], True, True, [b_q, b_k], [self.b_ps[2]])
                        kb.op("dve", [self.b_ps[1], b_b], [b_S], lambda E: E.tensor_tensor(
                            out=Sb[:, 0:512], in0=psA[:, 0:512], in1=bia[:, pat, 0:512], op=ALU.add))
                        kb.op("dve", [self.b_ps[2], b_b], [b_S], lambda E: E.tensor_tensor(
                            out=Sb[:, 512:576], in0=psB[:, 0:64], in1=bia[:, pat, 512:576], op=ALU.add))
                        kb.op("act", [self.b_ps[2]], [b_S], lambda E: E.activation(out=Sb[:, 576:832], in_=psB[:, 64:320], func=AF.Copy))
                        W = 832
                        blocks = [(ks + b, 128, b * 128) for b in range(4)] + [(ks + 4, 64, 512), (16, 128, 576), (17, 128, 704)]
                    else:
                        self.mm(psA[:, 0:256], q, kT[:, NL:NT], True, True, [b_q, b_k], [self.b_ps[1]])
                        kb.op("act", [self.b_ps[1]], [b_S], lambda E: E.activation(out=Sb[:, 0:256], in_=psA[:, 0:256], func=AF.Copy))
                        W = 256
                        blocks = [(16, 128, 0), (17, 128, 128)]
                    kb.op("dve", [b_S], [b_st], lambda E: E.tensor_reduce(out=st[:, 0:1], in_=Sb[:, :W], axis=AX.X, op=ALU.max))
                    kb.op("dve", [b_st], [b_st], lambda E: E.tensor_scalar(out=st[:, 1:2], in0=st[:, 0:1], scalar1=-1.0, scalar2=None, op0=ALU.mult))
                    kb.op("act", [b_S, b_st], [b_S], lambda E: E.activation(out=Sb[:, :W], in_=Sb[:, :W], func=AF.Exp, bias=st[:, 1:2], scale=1.0))
                    kb.op("dve", [b_S], [b_st], lambda E: E.tensor_reduce(out=st[:, 2:3], in_=Sb[:, :W], axis=AX.X, op=ALU.add))
                    kb.op("dve", [b_st], [b_st], lambda E: E.reciprocal(out=st[:, 3:4], in_=st[:, 2:3]))
                    kb.op("dve", [b_S, b_st], [b_P], lambda E: E.tensor_scalar(out=Pn[:, :W], in0=Sb[:, :W], scalar1=st[:, 3:4], scalar2=None, op0=ALU.mult))
                    for bi, (vt, nk, c0) in enumerate(blocks):
                        kb.op("pe", [b_P, self.b_const], [self.b_ps[3]], lambda E: E.transpose(
                            psTb[0:nk, bi * 128:(bi + 1) * 128], Pn[:, c0:c0 + nk], self.ident_b[:]))
                        kb.op("act", [self.b_ps[3]], [b_PT], lambda E: E.activation(
                            out=PT[0:nk, bi, :], in_=psTb[0:nk, bi * 128:(bi + 1) * 128], func=AF.Copy))
                    psO = self.psum[4]
                    for bi, (vt, nk, c0) in enumerate(blocks):
                        self.mm(psO[:, 0:128], va[0:nk, vt, :], PT[0:nk, bi, :], bi == 0, bi == len(blocks) - 1, [b_v, b_PT], [self.b_ps[4]])
                    kb.op("act", [self.b_ps[4]], [b_o], lambda E: E.activation(out=oTs[:, T * 128:(T + 1) * 128], in_=psO[:, 0:128], func=AF.Copy))
                kb.dma("sp", lambda: self.hd3(self.YC_d, 0, hh), oTs[:], [b_o], [b_yc])
            kb.barrier()

    def phase_mlstm(self, l, i, b_qr, b_vd, b_gd, b_yc):
        nc, kb = self.nc, self.kb
        from contextlib import ExitStack
        ORD = [[16, 17] + list(range(16)), [17, 16] + list(range(15, -1, -1))]
        with ExitStack() as es0:
            def sb0(name, shape, dt):
                return es0.enter_context(nc.sbuf_tensor(self.un(name), list(shape), dt))
            TOK = [sb0("l_tok%d" % d, [128, 18, 5, 4], F32) for d in range(2)]
            ROW = [[sb0("l_row%d%d" % (d, a), [4, NT], F32) for a in range(2)] for d in range(2)]
            FO = [sb0("l_fo%d" % d, [4, 36], F32) for d in range(2)]
            gst = sb0("l_gst", [128, 18, 4, 4], F32)
            gb = sb0("l_gb", [4, 4], F32)
            sel = sb0("l_sel", [4, 4, 128], F32)
            cwq = sb0("l_cwq", [128, 4, 3], F32)
            cwk = sb0("l_cwk", [128, 4, 3], F32)
            Jm = sb0("l_J", [128, 128], F32)
            mk = sb0("l_mk", [128, 2, 128], F32)
            hgb = sb0("l_hg", [128, 128], F32)
            b_tok, b_row, b_fo = [kb.buf(), kb.buf()], [kb.buf(), kb.buf()], [kb.buf(), kb.buf()]
            b_c = kb.buf()
            for T in range(18):
                kb.dma("sp", gst[:, T, :, :].rearrange("p y (s e) -> p y s e", s=1),
                       lambda: self.G_d[T * 128:(T + 1) * 128, :].rearrange("p (y s e) -> p y s e", y=4, s=2)[:, :, bass.ts(self.slot, 1), :],
                       [b_gd], [b_c])
            kb.dma("sp", gb[:], lambda: self.gate_bT[i].rearrange("(s e) y -> s e y", s=2)[bass.ts(self.slot, 1)].rearrange("s e y -> (s e) y"), [], [b_c])
            kb.dma("sp", sel[:], self.sel8[0:4, 0:4, :], [], [b_c])
            cv = self.convT[i].rearrange("p (w s e) k -> p w s e k", w=2, s=2)
            kb.dma("sp", cwq[:], lambda: cv[:, 0, bass.ts(self.slot, 1)].rearrange("p s e k -> p (s e) k"), [], [b_c])
            kb.dma("sp", cwk[:], lambda: cv[:, 1, bass.ts(self.slot, 1)].rearrange("p s e k -> p (s e) k"), [], [b_c])
            kb.dma("sp", Jm[:], self.Jm[:, :], [], [b_c])
            kb.dma("sp", mk[:], self.maskT.rearrange("a p t -> p a t"), [], [b_c])
            with ExitStack() as es:
                def sbt(name, shape, dt):
                    return es.enter_context(nc.sbuf_tensor(self.un(name), list(shape), dt))
                A = {k: sbt("l_" + k, [4, NT], F32) for k in ("IG", "LF", "CU", "B", "PM", "GX", "WL", "FL")}
                PE_ = sbt("l_pe", [4, 37], F32)
                t40 = sbt("l_t40", [128, 20], F32)
                bA = {k: kb.buf() for k in A}
                b_pe, b_t40 = kb.buf(), kb.buf()
                v3 = lambda ap: ap.rearrange("p (c s) -> p c s", s=64)
                for d in range(2):
                    flip = self.ident_f if d == 0 else Jm
                    for p in range(18):
                        T = ORD[d][p]
                        for gi, key in ((0, "IG"), (1, "LF")):
                            self.mm(self.psum[5][0:4, 0:128], gst[:, T, 2 * d + gi, :], flip[:], True, True, [b_c, self.b_const], [self.b_ps[5]])
                            kb.op("act", [self.b_ps[5], b_c], [bA[key]], lambda E: E.activation(
                                out=A[key][:, p * 128:(p + 1) * 128], in_=self.psum[5][0:4, 0:128], func=AF.Identity,
                                bias=gb[:, 2 * d + gi:2 * d + gi + 1], scale=1.0))
                    kb.op("act", [bA["LF"]], [bA["LF"]], lambda E: E.activation(out=A["LF"][:], in_=A["LF"][:], func=AF.Sigmoid))
                    kb.op("act", [bA["LF"]], [bA["LF"]], lambda E: E.activation(out=A["LF"][:], in_=A["LF"][:], func=AF.Ln))
                    kb.op("dve", [bA["LF"], self.b_const], [bA["CU"]], lambda E: E.tensor_tensor_scan(
                        out=A["CU"][:], data0=self.ones_f[0:4, 0:1].to_broadcast([4, NT]), data1=A["LF"][:], initial=0.0,
                        op0=ALU.mult, op1=ALU.add))
                    kb.op("dve", [bA["IG"], bA["CU"]], [bA["B"]], lambda E: E.tensor_tensor(out=A["B"][:], in0=A["IG"][:], in1=A["CU"][:], op=ALU.subtract))
                    kb.op("dve", [bA["B"]], [bA["PM"]], lambda E: E.tensor_tensor_scan(
                        out=A["PM"][:], data0=A["B"][:], data1=A["B"][:], initial=0.0, op0=ALU.max, op1=ALU.max))
                    kb.op("dve", [], [b_pe], lambda E: E.memset(PE_[:, 0:1], 0.0))
                    kb.op("dve", [bA["PM"]], [b_pe], lambda E: E.tensor_copy(out=PE_[:, 1:37], in_=v3(A["PM"][:])[:, :, 63]))
                    kb.op("dve", [b_pe, bA["PM"]], [bA["GX"]], lambda E: E.tensor_tensor(
                        out=v3(A["GX"][:]), in0=PE_[:, 0:36].unsqueeze(2).to_broadcast([4, 36, 64]), in1=v3(A["PM"][:]), op=ALU.subtract))
                    kb.op("act", [bA["GX"]], [bA["GX"]], lambda E: E.activation(out=A["GX"][:], in_=A["GX"][:], func=AF.Exp))
                    kb.op("dve", [b_pe, bA["B"]], [bA["WL"]], lambda E: E.tensor_tensor(
                        out=v3(A["WL"][:]), in0=v3(A["B"][:]), in1=PE_[:, 1:37].unsqueeze(2).to_broadcast([4, 36, 64]), op=ALU.subtract))
                    kb.op("act", [bA["WL"]], [bA["WL"]], lambda E: E.activation(out=A["WL"][:], in_=A["WL"][:], func=AF.Exp))
                    kb.op("dve", [b_pe], [b_fo[d]], lambda E: E.tensor_tensor(out=FO[d][:], in0=PE_[:, 0:36], in1=PE_[:, 1:37], op=ALU.subtract))
                    kb.op("act", [b_fo[d]], [b_fo[d]], lambda E: E.activation(out=FO[d][:], in_=FO[d][:], func=AF.Exp))
                    kb.op("dve", [bA["CU"], bA["PM"]], [bA["FL"]], lambda E: E.tensor_tensor(out=A["FL"][:], in0=A["CU"][:], in1=A["PM"][:], op=ALU.add))
                    kb.op("act", [bA["FL"]], [bA["FL"]], lambda E: E.activation(out=A["FL"][:], in_=A["FL"][:], func=AF.Exp, scale=-1.0))
                    kb.op("dve", [bA["PM"]], [bA["PM"]], lambda E: E.tensor_scalar(out=A["PM"][:], in0=A["PM"][:], scalar1=-1.0, scalar2=None, op0=ALU.mult))
                    for p in range(18):
                        T = ORD[d][p]
                        for a, key in enumerate(("B", "WL", "FL", "PM", "GX")):
                            self.mm(self.psum[5][:, a * 4:(a + 1) * 4], A[key][:, p * 128:(p + 1) * 128], self.ident_f[0:4, 0:4], True, True,
                                    [bA[key], self.b_const], [self.b_ps[5]])
                        kb.op("act", [self.b_ps[5]], [b_t40], lambda E: E.activation(out=t40[:], in_=self.psum[5][:, 0:20], func=AF.Copy))
                        self.mm(self.psum[6][:, 0:20], flip[:], t40[:], True, True, [b_c, self.b_const, b_t40], [self.b_ps[6]])
                        kb.op("act", [self.b_ps[6]], [b_tok[d]], lambda E: E.activation(
                            out=TOK[d][:, T, :, :].rearrange("p a b -> p (a b)"), in_=self.psum[6][:, 0:20], func=AF.Copy))
                        for a in range(2):
                            self.mm(self.psum[7][0:4, a * 128:(a + 1) * 128], TOK[d][:, T, 3 + a, :], self.ident_f[:], True, True,
                                    [b_tok[d], self.b_const], [self.b_ps[7]])
                            kb.op("act", [self.b_ps[7]], [b_row[d]], lambda E: E.activation(
                                out=ROW[d][a][:, T * 128:(T + 1) * 128], in_=self.psum[7][0:4, a * 128:(a + 1) * 128], func=AF.Copy))
                kb.barrier()
            with ExitStack() as es:
                def sbt(name, shape, dt):
                    return es.enter_context(nc.sbuf_tensor(self.un(name), list(shape), dt))
                qT = sbt("l_q", [128, NT], BF16)
                kT = sbt("l_k", [128, NT], BF16)
                Va = sbt("l_va", [128, 18, 132], BF16)
                so = sbt("l_so", [128, 18, 128], BF16)
                kw = [sbt("l_kw%d" % d, [128, 18, 128], BF16) for d in range(2)]
                qg = [sbt("l_qg%d" % d, [128, NT], BF16) for d in range(2)]
                hout = sbt("l_ho", [128, 18, 128], F32)
                ycs = sbt("l_yc", [128, NT], BF16)
                fob = [sbt("l_fob%d" % d, [128, 36], F32) for d in range(2)]
                Cst = [sbt("l_C%d" % d, [128, 132], F32) for d in range(2)]
                Cbf = [[sbt("l_Cb%d%d" % (d, k), [128, 132], BF16) for k in range(2)] for d in range(2)]
                Et = sbt("l_E", [128, 128], F32)
                wT = sbt("l_w", [128, 128], BF16)
                sm = sbt("l_sm", [128, 8], F32)
                yt = sbt("l_yt", [128, 128], F32)
                rq = sbt("l_rq", [128, NT], F32)
                cy = sbt("l_cy", [128, NT], F32)
                b_rq, b_cy = kb.buf(), kb.buf()
                b_q, b_k, b_va, b_so, b_ho, b_ycs, b_E, b_w, b_sm, b_yt = [kb.buf() for _ in range(10)]
                b_kw, b_qg, b_fob, b_C = [kb.buf(), kb.buf()], [kb.buf(), kb.buf()], [kb.buf(), kb.buf()], [kb.buf(), kb.buf()]
                b_Cb = [[kb.buf(), kb.buf()], [kb.buf(), kb.buf()]]
                psKb = self.psum[5][:].bitcast(BF16)
                for h in range(4):
                    for qi, (cwt, dst, b_dst) in enumerate(((cwq, qT, b_q), (cwk, kT, b_k))):
                        kb.dma("sp", rq[:], lambda: self.hd3(self.QR_d, qi, h), [b_qr], [b_rq])
                        kb.op("dve", [b_rq, b_c], [b_cy], lambda E: E.tensor_scalar(out=cy[:], in0=rq[:], scalar1=cwt[:, h, 1:2],
                                                                                  scalar2=None, op0=ALU.mult))
                        for (a_, b_) in ((0, NL), (NL, NT)):
                            kb.op("dve", [b_rq, b_c, b_cy], [b_cy], lambda E: E.scalar_tensor_tensor(
                                out=cy[:, a_ + 1:b_], in0=rq[:, a_:b_ - 1], scalar=cwt[:, h, 0:1], in1=cy[:, a_ + 1:b_], op0=ALU.mult, op1=ALU.add))
                            kb.op("dve", [b_rq, b_c, b_cy], [b_cy], lambda E: E.scalar_tensor_tensor(
                                out=cy[:, a_:b_ - 1], in0=rq[:, a_ + 1:b_], scalar=cwt[:, h, 2:3], in1=cy[:, a_:b_ - 1], op0=ALU.mult, op1=ALU.add))
                        if qi == 0:
                            kb.op("act", [b_cy], [b_dst], lambda E: E.activation(out=dst[:], in_=cy[:], func=AF.Silu))
                        else:
                            kb.op("act", [b_cy], [b_cy], lambda E: E.activation(out=cy[:], in_=cy[:], func=AF.Silu))
                            kb.op("pool", [b_cy], [b_dst], lambda E: E.tensor_scalar(out=dst[:], in0=cy[:], scalar1=128.0 ** -0.5,
                                                                                   scalar2=None, op0=ALU.mult))
                    kb.dma("sp", Va[:, :, 0:128], lambda: self.vcols(1, h), [b_vd], [b_va])
                    kb.op("dve", [b_va], [b_va], lambda E: E.memset(Va[:, :, 128:129], 1.0))
                    kb.dma("sp", so[:], lambda: self.vcols(2, h), [b_vd], [b_so])
                    kb.dma("sp", hgb[:, 0:128], lambda: self.head_gain[i:i + 1, :].rearrange("o (s e j) -> o s e j", s=2, e=4)[:, bass.ts(self.slot, 1), h, :]
                           .rearrange("o s j -> o (s j)").partition_broadcast(128), [], [b_c])
                    for d in range(2):
                        self.mm(self.psum[6][:, 0:36], sel[:, h, :], FO[d][:], True, True, [b_c, b_fo[d]], [self.b_ps[6]])
                        kb.op("act", [self.b_ps[6]], [b_fob[d]], lambda E: E.activation(out=fob[d][:], in_=self.psum[6][:, 0:36], func=AF.Copy))
                        for (t0, n) in ((0, 512), (512, 512), (1024, 512), (1536, 512), (2048, 256)):
                            self.mm(self.psum[7][:, :n], sel[:, h, :], ROW[d][1][:, t0:t0 + n], True, True, [b_c, b_row[d]], [self.b_ps[7]])
                            kb.op("dve", [self.b_ps[7], b_q], [b_qg[d]], lambda E: E.tensor_tensor(
                                out=qg[d][:, t0:t0 + n], in0=self.psum[7][:, :n], in1=qT[:, t0:t0 + n], op=ALU.mult))
                        kb.op("dve", [], [b_C[d]], lambda E: E.memset(Cst[d][:], 0.0))
                        kb.op("dve", [], [b_Cb[d][0]], lambda E: E.memset(Cbf[d][0][:], 0.0))
                    for T in range(18):
                        kb.op("pe", [b_k, self.b_const], [self.b_ps[5]], lambda E: E.transpose(
                            psKb[:, 0:128], kT[:, T * 128:(T + 1) * 128], self.ident_b[:]))
                        for d in range(2):
                            kb.op("dve", [self.b_ps[5], b_tok[d]], [b_kw[d]], lambda E: E.tensor_scalar(
                                out=kw[d][:, T, :], in0=psKb[:, 0:128], scalar1=TOK[d][:, T, 1, h:h + 1], scalar2=None, op0=ALU.mult))
                    for p in range(18):
                        for d in range(2):
                            T = ORD[d][p]
                            halves = (0, 1) if d == 0 else (1, 0)
                            first = (d == 0) == ((T + 2 if T < 16 else T - 16) <= (17 - T if T < 16 else 17 - T))
                            jF = T + 2 if T < 16 else T - 16
                            jR = 17 - T
                            first = (jF <= jR) if d == 0 else (jR < jF)
                            for hi, hf in enumerate(halves):
                                ch = 2 * p + hi
                                r0 = hf * 64
                                self.mm(self.psum[4][:, 0:129], kw[d][r0:r0 + 64, T, :], Va[r0:r0 + 64, T, 0:129], True, True,
                                        [b_kw[d], b_va], [self.b_ps[4]])
                                kb.op("dve", [self.b_ps[4], b_fob[d], b_C[d]], [b_C[d]], lambda E: E.scalar_tensor_tensor(
                                    out=Cst[d][:, 0:129], in0=Cst[d][:, 0:129], scalar=fob[d][:, ch:ch + 1], in1=self.psum[4][:, 0:129],
                                    op0=ALU.mult, op1=ALU.add))
                                if hi == 0:
                                    kb.op("act", [b_C[d]], [b_Cb[d][1]], lambda E: E.activation(
                                        out=Cbf[d][1][:, 0:129], in_=Cst[d][:, 0:129], func=AF.Copy))
                                if hi == 0:
                                    tc = slice(T * 128, (T + 1) * 128)
                                    self.mm(self.psum[1][:, 0:128], kT[:, tc], qT[:, tc], True, True, [b_k, b_q], [self.b_ps[1]])
                                    self.mm(self.psum[2][:, 0:128], sel[:, h, :], ROW[d][0][:, tc], True, False, [b_c, b_row[d]], [self.b_ps[2]])
                                    self.mm(self.psum[2][:, 0:128], self.ident_f[:], mk[:, d, :], False, True, [b_c, self.b_const], [self.b_ps[2]])
                                    kb.op("act", [self.b_ps[2], b_tok[d]], [b_E], lambda E: E.activation(
                                        out=Et[:], in_=self.psum[2][:, 0:128], func=AF.Exp, bias=TOK[d][:, T, 0, h:h + 1], scale=1.0))
                                    kb.op("dve", [self.b_ps[1], b_E], [b_w], lambda E: E.tensor_tensor(
                                        out=wT[:], in0=self.psum[1][:, 0:128], in1=Et[:], op=ALU.mult))
                                    self.mm(self.psum[3][:, 0:129], wT[:], Va[:, T, 0:129], True, False, [b_w, b_va], [self.b_ps[3]])
                            for hi, hf in enumerate(halves):
                                r0 = hf * 64
                                self.mm(self.psum[3][r0:r0 + 64, 0:129], qg[d][:, T * 128 + r0:T * 128 + r0 + 64], Cbf[d][hi][:, 0:129],
                                        False, hi == 1, [b_qg[d], b_Cb[d][hi]], [self.b_ps[3]])
                            kb.op("act", [self.b_ps[3]], [b_sm], lambda E: E.activation(out=sm[:, 0:1], in_=self.psum[3][:, 128:129], func=AF.Abs))
                            kb.op("dve", [b_sm, b_tok[d]], [b_sm], lambda E: E.tensor_tensor(out=sm[:, 1:2], in0=sm[:, 0:1], in1=TOK[d][:, T, 2, h:h + 1], op=ALU.max))
                            kb.op("dve", [b_sm], [b_sm], lambda E: E.reciprocal(out=sm[:, 2:3], in_=sm[:, 1:2]))
                            if first:
                                kb.op("dve", [self.b_ps[3], b_sm], [b_ho], lambda E: E.tensor_scalar(
                                    out=hout[:, T, :], in0=self.psum[3][:, 0:128], scalar1=sm[:, 2:3], scalar2=None, op0=ALU.mult))
                            else:
                                kb.op("dve", [self.b_ps[3], b_sm, b_ho], [b_ho], lambda E: E.scalar_tensor_tensor(
                                    out=hout[:, T, :], in0=self.psum[3][:, 0:128], scalar=sm[:, 2:3], in1=hout[:, T, :], op0=ALU.mult, op1=ALU.add))
                            kb.op("act", [b_C[d]], [b_Cb[d][0]], lambda E: E.activation(out=Cbf[d][0][:, 0:129], in_=Cst[d][:, 0:129], func=AF.Copy))
                    for T in range(18):
                        kb.op("act", [b_ho], [b_yt, b_sm], lambda E: E.activation(out=yt[:], in_=hout[:, T, :], func=AF.Square, accum_out=sm[:, 4:5]))
                        kb.op("act", [b_sm], [b_sm], lambda E: E.activation(out=sm[:, 5:6], in_=sm[:, 4:5], func=AF.Sqrt, bias=EPS, scale=1.0 / 128))
                        kb.op("dve", [b_sm], [b_sm], lambda E: E.reciprocal(out=sm[:, 6:7], in_=sm[:, 5:6]))
                        kb.op("dve", [b_ho, b_sm, b_c], [b_yt], lambda E: E.scalar_tensor_tensor(
                            out=yt[:], in0=hout[:, T, :], scalar=sm[:, 6:7], in1=hgb[:, 0:128], op0=ALU.mult, op1=ALU.mult))
                        kb.op("dve", [b_yt, b_so], [b_yt], lambda E: E.tensor_tensor(out=yt[:], in0=yt[:], in1=so[:, T, :], op=ALU.mult))
                        self.mm(self.psum[6][:, 0:128], yt[:], self.ident_f[:], True, True, [b_yt, self.b_const], [self.b_ps[6]])
                        kb.op("act", [self.b_ps[6]], [b_ycs], lambda E: E.activation(out=ycs[:, T * 128:(T + 1) * 128], in_=self.psum[6][:, 0:128], func=AF.Copy))
                    kb.dma("sp", lambda: self.hd3(self.YC_d, 1, h), ycs[:], [b_ycs], [b_yc])
                kb.barrier()

    def phase_odd(self, l, need_ctx):
        nc, kb = self.nc, self.kb
        from contextlib import ExitStack
        i = l // 2
        groups = [self.LG[0], self.LG[1]] + ([self.LG[2]] if need_ctx else [])
        w_in, w_out = self.odd_w_in[i], self.odd_w_out[i]
        for (t0, n, col, g) in groups:
            nT = n // 128
            with ExitStack() as es:
                def sbt(name, shape, dt):
                    return es.enter_context(nc.sbuf_tensor(self.un(name), list(shape), dt))
                hg = sbt("s_h", [128, NCH, 512], F32)
                xm = sbt("s_xm", [128, NCH, 512], BF16)
                vt = sbt("s_vt", [128, 4, 4096], BF16)
                pr = sbt("s_pr", [128, 32, 512], BF16)
                wv = [sbt("s_wv%d" % k, [128, NCH, 256], BF16) for k in range(2)]
                wu = [sbt("s_wu%d" % k, [128, NCH, 128], BF16) for k in range(2)]
                wo = [sbt("s_wo%d" % k, [128, 32, 128], BF16) for k in range(2)]
                gt = [sbt("s_g%d" % k, [128, 512], F32) for k in range(4)]
                wsT = sbt("s_wsT", [128, 8, 128], F32)
                wsc = sbt("s_wsc", [128, 4, 8, 128], BF16)
                bbc = sbt("s_bbc", [128, 8, 128], F32)
                vg = sbt("s_vg", [128, 32], F32)
                ss = sbt("s_ss", [128, 4, 16], F32)
                rs = sbt("s_rs", [128, 8], F32)
                tmp, b_tmp = self.norm_tmp(sbt)
                b_h, b_xm, b_vt, b_pr, b_c, b_ss, b_rs, b_wsc = [kb.buf() for _ in range(8)]
                b_wv, b_wu, b_wo = [kb.buf(), kb.buf()], [kb.buf(), kb.buf()], [kb.buf(), kb.buf()]
                b_g = [kb.buf() for _ in range(4)]
                kb.dma("sp", wsT[:], self.odd_wsT[i], [], [b_c])
                kb.dma("sp", bbc[:].rearrange("p a b -> p (a b)"), self.odd_bs[i:i + 1, :].partition_broadcast(128), [], [b_c])
                kb.dma("sp", vg[:], self.odd_vg[i], [], [b_c])
                kb.dma("sp", hg[:, :, :n], lambda: self.tk(self.hT_d, g), [self.b_hT[g]], [b_h])
                kb.op("dve", [], [b_ss], lambda E: E.memset(ss[:], 0.0))
                self.norm_tile(l, 0, col, hg[:, :, :n], n, xm[:, :, :n], b_h, b_xm, tmp, b_tmp, 0)

                def gelu(ps_ap, bps, m, out_f32_idx):
                    a, r = gt[1], gt[out_f32_idx]
                    kb.op("act", [bps], [b_g[1]], lambda E: E.activation(out=a[:, :m], in_=ps_ap, func=AF.Square))
                    kb.op("pool", [b_g[1]], [b_g[1]], lambda E: E.tensor_scalar(out=a[:, :m], in0=a[:, :m], scalar1=0.044715,
                                                                               scalar2=1.0, op0=ALU.mult, op1=ALU.add))
                    kb.op("dve", [bps, b_g[1]], [b_g[0]], lambda E: E.tensor_tensor(out=gt[0][:, :m], in0=ps_ap, in1=a[:, :m], op=ALU.mult))
                    kb.op("act", [b_g[0]], [b_g[0]], lambda E: E.activation(out=gt[0][:, :m], in_=gt[0][:, :m], func=AF.Sigmoid,
                                                                           scale=1.5957691216057308))
                    kb.op("dve", [bps, b_g[0]], [b_g[out_f32_idx]], lambda E: E.tensor_tensor(
                        out=r[:, :m], in0=ps_ap, in1=gt[0][:, :m], op=ALU.mult))
                for ct in range(16):
                    s_ = ct % 2
                    c0 = 4096 + ct * 256
                    kb.dma("pool", wv[s_][:], w_in.rearrange("(k p) f -> p k f", p=128)[:, :, c0:c0 + 256], [], [b_wv[s_]])
                    for T in range(nT):
                        pb = 1 + (ct * nT + T) % 2
                        ps = self.psum[pb]
                        for k in range(NCH):
                            self.mm(ps[:, 0:256], xm[:, k, T * 128:(T + 1) * 128], wv[s_][:, k, :], k == 0, k == NCH - 1,
                                    [b_xm, b_wv[s_]], [self.b_ps[pb]])
                        gelu(ps[:, 0:256], self.b_ps[pb], 256, 2)
                        kb.op("act", [b_g[2]], [b_g[1], b_ss], lambda E: E.activation(
                            out=gt[1][:, :256], in_=gt[2][:, :256], func=AF.Square, accum_out=ss[:, T, ct:ct + 1]))
                        kb.op("pool", [b_g[2]], [b_vt], lambda E: E.tensor_copy(
                            out=vt[:, T, ct * 256:(ct + 1) * 256], in_=gt[2][:, :256]))
                kb.op("dve", [b_ss], [b_rs], lambda E: E.tensor_reduce(out=rs[:, 0:4], in_=ss[:], axis=AX.X, op=ALU.add))
                kb.op("act", [b_rs], [b_rs], lambda E: E.activation(out=rs[:, 0:4], in_=rs[:, 0:4], func=AF.Sqrt,
                                                                    bias=EPS, scale=1.0 / 4096))
                kb.op("dve", [b_rs], [b_rs], lambda E: E.reciprocal(out=rs[:, 4:8], in_=rs[:, 0:4]))
                for T in range(nT):
                    kb.op("dve", [b_rs, b_c], [b_wsc], lambda E: E.tensor_scalar(
                        out=wsc[:, T, :, :], in0=wsT[:], scalar1=rs[:, 4 + T:5 + T], scalar2=None, op0=ALU.mult))
                for j in range(32):
                    s_ = j % 2
                    gi = j // 4
                    kb.dma("pool", wu[s_][:], w_in.rearrange("(k p) f -> p k f", p=128)[:, :, j * 128:(j + 1) * 128], [], [b_wu[s_]])
                    psU = self.psum[3 + s_]
                    for k in range(NCH):
                        self.mm(psU[:, :n], wu[s_][:, k, :], xm[:, k, :n], k == 0, k == NCH - 1, [b_wu[s_], b_xm], [self.b_ps[3 + s_]])
                    gelu(psU[:, :n], self.b_ps[3 + s_], n, 2)
                    psS = self.psum[5 + s_]
                    for T in range(nT):
                        self.mm(psS[:, T * 128:(T + 1) * 128], vt[:, T, j * 128:(j + 1) * 128], wsc[:, T, gi, :], True, True,
                                [b_vt, b_wsc], [self.b_ps[5 + s_]])
                    kb.op("dve", [self.b_ps[5 + s_], b_c], [b_g[3]], lambda E: E.scalar_tensor_tensor(
                        out=gt[3][:, :n].rearrange("p (a b) -> p a b", b=128), in0=psS[:, :n].rearrange("p (a b) -> p a b", b=128),
                        scalar=vg[:, j:j + 1], in1=bbc[:, gi:gi + 1, :].to_broadcast([128, nT, 128]),
                        op0=ALU.mult, op1=ALU.add))
                    kb.op("pool", [b_g[3], b_g[2]], [b_pr], lambda E: E.tensor_tensor(
                        out=pr[:, j, :n], in0=gt[3][:, :n], in1=gt[2][:, :n], op=ALU.mult))
                for dc in range(NCH):
                    s_ = dc % 2
                    kb.dma("pool", wo[s_][:], w_out.rearrange("(k p) d -> p k d", p=128)[:, :, dc * 128:(dc + 1) * 128], [], [b_wo[s_]])
                    psO = self.psum[1 + s_]
                    for k in range(32):
                        self.mm(psO[:, :n], wo[s_][:, k, :], pr[:, k, :n], k == 0, k == 31, [b_wo[s_], b_pr], [self.b_ps[1 + s_]])
                    kb.op("dve", [self.b_ps[1 + s_], b_h, self.b_modT], [b_h], lambda E: E.scalar_tensor_tensor(
                        out=hg[:, dc, :n], in0=psO[:, :n], scalar=self.modT[:, l, 2 * 16 + dc, col:col + 1],
                        in1=hg[:, dc, :n], op0=ALU.mult, op1=ALU.add))
                kb.dma("sp", lambda: self.tk(self.hT_d, g), hg[:, :, :n], [b_h], [self.b_hT[g]])
                kb.barrier()

    def phase_moe(self, l, need_ctx):
        nc, kb = self.nc, self.kb
        from contextlib import ExitStack
        for p in range(1):
            with ExitStack() as es:
                def sbt(name, shape, dt):
                    return es.enter_context(nc.sbuf_tensor(self.un(name), list(shape), dt))
                subs = [self.LG[0], self.LG[1]] + ([self.LG[2]] if need_ctx else [])
                hh = sbt("m_h", [128, NCH, 1152], F32)
                xm2 = sbt("m_xm2", [128, NCH, 1152], BF16)
                wr_s = sbt("m_wr", [128, NCH, 36], F32)
                br_s = sbt("m_br", [1, 36], F32)
                sel_s = sbt("m_sel", [32, 32, 128], BF16)
                combT = sbt("m_combT", [32, 1152], BF16)
                rt = sbt("m_rt", [128, 8, 40], F32)
                wg = [sbt("m_wg%d" % i, [128, NCH, 256], BF16) for i in range(2)]
                wu = [sbt("m_wu%d" % i, [128, NCH, 256], BF16) for i in range(2)]
                wd = [sbt("m_wd%d" % i, [128, 2, D], BF16) for i in range(2)]
                t1 = [sbt("m_t1%d" % i, [128, 512], F32) for i in range(2)]
                hid0 = [sbt("m_hid%d" % f, [128, 512], BF16) for f in range(2)]
                hid = [hid0, hid0]
                tmp, b_tmp = self.norm_tmp(sbt)
                t2 = tmp["t1"]
                xf = t1
                b_hh = [kb.buf() for _ in subs]
                b_xm = [kb.buf() for _ in subs]
                b_wr, b_sel, b_comb, b_rt = kb.buf(), kb.buf(), kb.buf(), kb.buf()
                b_wg, b_wu, b_wd = [kb.buf(), kb.buf()], [kb.buf(), kb.buf()], [kb.buf(), kb.buf()]
                b_t1, b_t2 = [kb.buf(), kb.buf()], [b_tmp["t10"], b_tmp["t11"]]
                b_xf = b_t1
                bh0 = [kb.buf(), kb.buf()]
                b_hid = [bh0, bh0]
                kb.dma("sp", wr_s[:], self.wr[l].rearrange("(k p) n -> p k n", p=128), [], [b_wr])
                kb.dma("sp", br_s[:], self.br[l:l + 1, :], [], [b_wr])
                kb.dma("sp", sel_s[:], self.sel32[:, :, :], [], [b_sel])
                offs = []
                o = 0
                for (t0, n, col, g) in subs:
                    offs.append(o)
                    o += n
                for si, (t0, n, col, g) in enumerate(subs):
                    o = offs[si]
                    kb.dma("sp", hh[:, :, o:o + n], lambda: self.tk(self.hT_d, g), [self.b_hT[g]], [b_hh[si]])
                    nj = n // 128

                    def cb(c, xf_t, b_xf_t, o=o, nj=nj):
                        for j in range(nj):
                            self.mm(self.psum[4 + j][:, 0:36], xf_t[:, j * 128:(j + 1) * 128], wr_s[:, c, :],
                                    c == 0, False, [b_xf_t, b_wr], [self.b_ps[4 + j]])
                    self.norm_tile(l, 1, col, hh[:, :, o:o + n], n, xm2[:, :, o:o + n], b_hh[si], b_xm[si],
                                   tmp, b_tmp, 0, f32_out=xf, b_f32=b_xf, chunk_cb=cb)
                    for j in range(nj):
                        psr = self.psum[4 + j]
                        self.mm(psr[:, 0:36], self.ones_f[0:1, 0:128], br_s[0:1, :], False, True,
                                [self.b_const, b_wr], [self.b_ps[4 + j]])
                        L = rt[:, 0, 0:36]
                        R = lambda k, a, b: rt[:, k, a:b]
                        V = lambda reads, fn: kb.op("dve", reads, [b_rt], fn)
                        V([self.b_ps[4 + j]], lambda E: E.tensor_copy(out=L, in_=psr[:, 0:36]))
                        V([b_rt], lambda E: E.tensor_reduce(out=R(1, 0, 1), in_=rt[:, 0, 0:4], axis=AX.X, op=ALU.max))
                        V([b_rt], lambda E: E.tensor_scalar(out=R(1, 4, 8), in0=rt[:, 0, 0:4], scalar1=R(1, 0, 1),
                                                            scalar2=None, op0=ALU.is_equal))
                        V([b_rt], lambda E: E.tensor_scalar(out=R(1, 1, 2), in0=R(1, 0, 1), scalar1=-1.0,
                                                            scalar2=None, op0=ALU.mult))
                        kb.op("act", [b_rt], [b_rt], lambda E: E.activation(out=R(1, 8, 12), in_=rt[:, 0, 0:4],
                                                                            func=AF.Exp, bias=R(1, 1, 2), scale=1.0))
                        V([b_rt], lambda E: E.tensor_reduce(out=R(1, 2, 3), in_=R(1, 8, 12), axis=AX.X, op=ALU.add))
                        V([b_rt], lambda E: E.tensor_scalar(out=R(1, 12, 16), in0=R(1, 4, 8), scalar1=-1.0, scalar2=1e30,
                                                            op0=ALU.add, op1=ALU.mult))
                        V([b_rt], lambda E: E.tensor_tensor(
                            out=rt[:, 2, 0:32].rearrange("p (g e) -> p g e", e=8),
                            in0=rt[:, 0, 4:36].rearrange("p (g e) -> p g e", e=8),
                            in1=R(1, 12, 16).unsqueeze(2).to_broadcast([128, 4, 8]), op=ALU.add))
                        V([b_rt], lambda E: E.tensor_reduce(out=R(1, 16, 17), in_=rt[:, 2, 0:32], axis=AX.X, op=ALU.max))
                        V([b_rt], lambda E: E.tensor_scalar(out=rt[:, 3, 0:32], in0=rt[:, 2, 0:32], scalar1=R(1, 16, 17),
                                                            scalar2=-1e30, op0=ALU.is_equal, op1=ALU.mult))
                        V([b_rt], lambda E: E.tensor_tensor(out=rt[:, 3, 0:32], in0=rt[:, 3, 0:32], in1=rt[:, 2, 0:32],
                                                            op=ALU.add))
                        V([b_rt], lambda E: E.tensor_reduce(out=R(1, 17, 18), in_=rt[:, 3, 0:32], axis=AX.X, op=ALU.max))
                        V([b_rt], lambda E: E.tensor_scalar(out=rt[:, 4, 0:32], in0=rt[:, 2, 0:32], scalar1=R(1, 17, 18),
                                                            scalar2=None, op0=ALU.is_ge))
                        V([b_rt], lambda E: E.tensor_scalar(out=R(1, 18, 19), in0=R(1, 16, 17), scalar1=-1.0,
                                                            scalar2=None, op0=ALU.mult))
                        kb.op("act", [b_rt], [b_rt], lambda E: E.activation(out=rt[:, 5, 0:32], in_=rt[:, 2, 0:32],
                                                                            func=AF.Exp, bias=R(1, 18, 19), scale=1.0))
                        V([b_rt], lambda E: E.tensor_tensor(out=rt[:, 5, 0:32], in0=rt[:, 5, 0:32], in1=rt[:, 4, 0:32],
                                                            op=ALU.mult))
                        V([b_rt], lambda E: E.tensor_reduce(out=R(1, 19, 20), in_=rt[:, 5, 0:32], axis=AX.X, op=ALU.add))
                        V([b_rt], lambda E: E.tensor_tensor(out=R(1, 20, 21), in0=R(1, 19, 20), in1=R(1, 2, 3), op=ALU.mult))
                        V([b_rt], lambda E: E.reciprocal(out=R(1, 21, 22), in_=R(1, 20, 21)))
                        V([b_rt], lambda E: E.tensor_scalar(out=rt[:, 6, 0:32], in0=rt[:, 5, 0:32], scalar1=R(1, 21, 22),
                                                            scalar2=None, op0=ALU.mult))
                        self.mm(self.psum[1][0:32, 0:128], rt[:, 6, 0:32], self.ident_f[:], True, True,
                                [b_rt, self.b_const], [self.b_ps[1]])
                        kb.op("act", [self.b_ps[1]], [b_comb], lambda E: E.activation(
                            out=combT[:, o + j * 128:o + (j + 1) * 128], in_=self.psum[1][0:32, 0:128], func=AF.Copy))
                it = 0
                for e in range(32):
                    for hf in range(2):
                        s = it % 2
                        it += 1
                        f0 = hf * 256
                        kb.dma("pool", wg[s][:], self.w_gate[l, e].rearrange("(k p) f -> p k f", p=128)[:, :, f0:f0 + 256],
                               [], [b_wg[s]])
                        kb.dma("pool", wu[s][:], self.w_up[l, e].rearrange("(k p) f -> p k f", p=128)[:, :, f0:f0 + 256],
                               [], [b_wu[s]])
                        kb.dma("pool", wd[s][:], self.w_down[l, e, f0:f0 + 256, :].rearrange("(k p) d -> p k d", p=128),
                               [], [b_wd[s]])
                        for si, (t0, n, col, g) in enumerate(subs):
                            o = offs[si]
                            hs = si % 2
                            psC = self.psum[4]
                            self.mm(psC[:, :n], sel_s[:, e, :], combT[:, o:o + n], True, True, [b_sel, b_comb], [self.b_ps[4]])
                            for fc in range(2):
                                psG, psU = self.psum[fc], self.psum[2 + fc]
                                for k in range(NCH):
                                    self.mm(psG[:, :n], wg[s][:, k, fc * 128:(fc + 1) * 128], xm2[:, k, o:o + n],
                                            k == 0, k == NCH - 1, [b_wg[s], b_xm[si]], [self.b_ps[fc]])
                                for k in range(NCH):
                                    self.mm(psU[:, :n], wu[s][:, k, fc * 128:(fc + 1) * 128], xm2[:, k, o:o + n],
                                            k == 0, k == NCH - 1, [b_wu[s], b_xm[si]], [self.b_ps[2 + fc]])
                                kb.op("act", [self.b_ps[fc]], [b_t1[fc]], lambda E: E.activation(
                                    out=t1[fc][:, :n], in_=psG[:, :n], func=AF.Silu))
                                kb.op("dve", [self.b_ps[2 + fc], b_t1[fc]], [b_t2[fc]], lambda E: E.tensor_tensor(
                                    out=t2[fc][:, :n], in0=psU[:, :n], in1=t1[fc][:, :n], op=ALU.mult))
                                kb.op("dve", [self.b_ps[4], b_t2[fc]], [b_hid[hs][fc]], lambda E: E.tensor_tensor(
                                    out=hid[hs][fc][:, :n], in0=psC[:, :n], in1=t2[fc][:, :n], op=ALU.mult))
                            for dc in range(NCH):
                                pb = 5 + dc % 3
                                psO = self.psum[pb]
                                for fc in range(2):
                                    self.mm(psO[:, :n], wd[s][:, fc, dc * 128:(dc + 1) * 128], hid[hs][fc][:, :n],
                                            fc == 0, fc == 1, [b_wd[s], b_hid[hs][fc]], [self.b_ps[pb]])
                                kb.op("dve", [self.b_ps[pb], b_hh[si], self.b_modT], [b_hh[si]],
                                      lambda E: E.scalar_tensor_tensor(
                                          out=hh[:, dc, o:o + n], in0=psO[:, :n], scalar=self.modT[:, l, 5 * 16 + dc, col:col + 1],
                                          in1=hh[:, dc, o:o + n], op0=ALU.mult, op1=ALU.add))
                for si, (t0, n, col, g) in enumerate(subs):
                    o = offs[si]
                    kb.dma("sp", lambda: self.tk(self.hT_d, g), hh[:, :, o:o + n], [b_hh[si]], [self.b_hT[g]])
                kb.barrier()

    def phase_mod(self, cT, w_mod, b_mod):
        nc, kb = self.nc, self.kb
        from contextlib import ExitStack
        with ExitStack() as es:
            def sbt(name, shape, dt):
                return es.enter_context(nc.sbuf_tensor(self.un(name), list(shape), dt))
            cT_s = sbt("cT_s", [128, NCH, 2], F32)
            sT = sbt("sT", [128, NCH, 2], BF16)
            modrow = sbt("modrow", [2, 6 * D], F32)
            brow = sbt("brow", [2, 6 * D], F32)
            wsl = [sbt("wmod_sl%d" % i, [128, NCH, 512], BF16) for i in range(2)]
            b_c, b_s, b_mr, b_br = kb.buf(), kb.buf(), kb.buf(), kb.buf()
            b_w = [kb.buf(), kb.buf()]
            kb.dma("sp", cT_s[:], cT[:, :, :], [], [b_c])
            kb.op("act", [b_c], [b_s], lambda E: E.activation(out=sT[:], in_=cT_s[:], func=AF.Silu))
            for l in range(DEPTH):
                for r in range(2):
                    kb.dma("sp", brow[r:r + 1, :], b_mod[l:l + 1, :], [], [b_br])
                wv = w_mod[l].rearrange("(k p) n -> p k n", p=128)
                for j in range(24):
                    s = j % 2
                    kb.dma("pool", wsl[s][:], wv[:, :, j * 512:(j + 1) * 512], [], [b_w[s]])
                    ps = self.psum[s]
                    for k in range(NCH):
                        self.mm(ps[0:2, :], sT[:, k, :], wsl[s][:, k, :], k == 0, k == NCH - 1,
                                [b_s, b_w[s]], [self.b_ps[s]])
                    kb.op("dve", [self.b_ps[s], b_br], [b_mr], lambda E: E.tensor_tensor(
                        out=modrow[:, j * 512:(j + 1) * 512], in0=ps[0:2, :], in1=brow[:, j * 512:(j + 1) * 512],
                        op=ALU.add))
                pst = self.psum[2]
                for cc in range(96):
                    self.mm(pst[:, cc * 2:cc * 2 + 2], modrow[0:2, cc * 128:(cc + 1) * 128], self.ident_f[0:2, 0:2],
                            True, True, [b_mr, self.b_const], [self.b_ps[2]])
                kb.op("dve", [self.b_ps[2]], [self.b_modT], lambda E: E.tensor_copy(
                    out=self.modT[:, l, :, :], in_=pst[:, 0:192].rearrange("p (a b) -> p a b", b=2)))
            kb.barrier()

    def phase_norm_test(self):
        pass


def expand_bias(rpb):
    L, H = rpb.shape[0], rpb.shape[1]
    flat = np.concatenate([rpb.reshape(L, H, -1), np.full((L, H, 1), NEG, np.float32)], axis=2)
    idx = np.full((5, 128, 576), 15 * 31, np.int64)
    pats = [(0, 0), (2, 0), (None, None), (28, 24), (30, 24)]
    for pi, (rq0, k0) in enumerate(pats):
        if rq0 is None:
            rq0, k0 = 8, 4
        for ql in range(128):
            r = rq0 + ql // 64
            cq = ql % 64
            r0 = min(max(r - 4, 0), 24)
            ws = min(max(cq - 8, 0), 48)
            for krl in range(9):
                kr = k0 + krl
                if not (r0 <= kr < r0 + 8) or kr > 31:
                    continue
                for ck in range(ws, ws + 16):
                    dc = min(max(ck - cq + 15, 0), 30)
                    idx[pi, ql, krl * 64 + ck] = (kr - r + 7) * 31 + dc
    out = flat[:, :, idx]
    return np.ascontiguousarray(out.astype(np.float32))


def host_prep(inputs):
    x = np.asarray(inputs["x"], np.float32)
    ctx = np.asarray(inputs["ctx"], np.float32)
    c = np.asarray(inputs["c"], np.float32)
    c_ctx = np.asarray(inputs["c_ctx"], np.float32)
    shared = {}
    shared["w_mod"] = np.ascontiguousarray(inputs["w_mod"], np.float32)
    shared["b_mod"] = np.ascontiguousarray(inputs["b_mod"], np.float32)
    ng = np.asarray(inputs["norm_gain"], np.float32)
    shared["ngT"] = np.ascontiguousarray(ng.reshape(DEPTH, 2, NCH, 128).transpose(3, 0, 1, 2))
    shared["wr"] = np.ascontiguousarray(np.concatenate([inputs["moe_w_rg"], inputs["moe_w_re"]], axis=2), np.float32)
    shared["br"] = np.ascontiguousarray(np.concatenate([inputs["moe_b_rg"], inputs["moe_b_re"]], axis=1), np.float32)
    for k in ("moe_w_gate", "moe_w_up", "moe_w_down"):
        shared[k] = np.ascontiguousarray(inputs[k], np.float32)
    import ml_dtypes
    sel = np.zeros((32, 32, 128), ml_dtypes.bfloat16)
    for e in range(32):
        sel[e, e, :] = 1.0
    shared["sel32"] = sel
    shared["odd_w_in"] = np.ascontiguousarray(inputs["odd_w_in"], np.float32)
    shared["odd_w_out"] = np.ascontiguousarray(inputs["odd_w_out"], np.float32)
    shared["odd_wsT"] = np.ascontiguousarray(np.asarray(inputs["odd_w_s"], np.float32).transpose(0, 3, 1, 2))
    shared["odd_bs"] = np.ascontiguousarray(np.asarray(inputs["odd_b_s"], np.float32).reshape(2, 1024))
    shared["odd_vg"] = np.ascontiguousarray(np.asarray(inputs["odd_v_gain"], np.float32).reshape(2, 32, 128).transpose(0, 2, 1))
    shared["even_w_in"] = np.ascontiguousarray(inputs["even_w_in"], np.float32)
    shared["even_w_out"] = np.ascontiguousarray(inputs["even_w_out"], np.float32)
    ec = np.asarray(inputs["even_conv"], np.float32)
    shared["convT"] = np.ascontiguousarray(ec.reshape(2, 3, 16, 128).transpose(0, 3, 2, 1))
    shared["qkgT"] = np.ascontiguousarray(np.asarray(inputs["even_qk_gain"], np.float32).transpose(0, 2, 1))
    shared["gate_bT"] = np.ascontiguousarray(np.asarray(inputs["even_gate_b"], np.float32).reshape(2, 4, 8).transpose(0, 2, 1))
    shared["head_gain"] = np.ascontiguousarray(inputs["even_head_gain"], np.float32)
    shared["biasx"] = expand_bias(np.asarray(inputs["even_rpb"], np.float32))
    t = np.arange(128)
    same = (t[:, None] // 64) == (t[None, :] // 64)
    mF = np.where(same & (t[:, None] <= t[None, :]), 0.0, NEG).astype(np.float32)
    mR = np.where(same & (t[:, None] >= t[None, :]), 0.0, NEG).astype(np.float32)
    shared["maskT"] = np.stack([mF, mR])
    s8 = np.zeros((8, 8, 128), np.float32)
    for e in range(8):
        s8[e, e, :] = 1.0
    shared["sel8"] = s8
    shared["Jm"] = np.ascontiguousarray(np.eye(128, dtype=np.float32)[::-1])
    nonce = np.full((1, 4), int(np.random.randint(1, 2 ** 30)), np.int32)
    shared["nonce"] = nonce
    per = []
    for core in range(8):
        b, sl = core // 2, core % 2
        tok = np.concatenate([x[b, sl * 1024:(sl + 1) * 1024], ctx[b, sl * 128:(sl + 1) * 128]], axis=0)
        xT = np.ascontiguousarray(tok.reshape(NLOC, NCH, 128).transpose(2, 1, 0))
        cc = np.stack([c[b], c_ctx], axis=1)
        cT = np.ascontiguousarray(cc.reshape(NCH, 128, 2).transpose(1, 0, 2))
        d = dict(shared)
        d["xT"] = xT
        d["cT"] = cT
        per.append(d)
    return per


def run(inputs, upto="all", ncores=8, trace=False):
    p = Prog(upto)
    p.build()
    per = host_prep(inputs)
    in_maps = [{k: per[i][k] for k in p.T} for i in range(ncores)]
    res = run_bass_kernel_spmd(p.nc, in_maps, core_ids=list(range(ncores)), trace=trace)
    return res


def kernel(**inputs):
    res = run(inputs, ncores=8)
    outp = np.empty((4, NL, D), np.float32)
    for core in range(8):
        b, sl = core // 2, core % 2
        o = np.asarray(res.results[core]["out"])
        outp[b, sl * 1024:(sl + 1) * 1024] = o.transpose(2, 1, 0).reshape(1024, D)
    return outp
```
